# Optimizing a Trainium2 kernel written in Bass

```python
import jax, jax.numpy as jnp
from jax import lax
import numpy as np

D_MODEL = 1024
BATCH = 8
SEQ = 4096
DEPTH = 1

HEAD_DIM = 64
N_HEADS = D_MODEL // HEAD_DIM
N_HEADS_A = N_HEADS // 2
N_HEADS_B = N_HEADS - N_HEADS_A
DILATED_CONFIGS = ((128, 1), (512, 4), (2048, 16))
BLOCK = 128
KV_RANK = D_MODEL // 8
IDX_HEADS = 8
IDX_DIM = 64
IDX_SCALE = (IDX_HEADS * IDX_DIM) ** -0.5
TOPK_MAX = 256
D_FF = -(-8 * D_MODEL // (3 * 256)) * 256
EPS = 1e-6
SPLITS = (N_HEADS_A * HEAD_DIM, N_HEADS_A * HEAD_DIM, N_HEADS_A * HEAD_DIM,
          N_HEADS_B * HEAD_DIM, KV_RANK,
          IDX_HEADS * IDX_DIM, IDX_DIM, IDX_HEADS)
D_IN = sum(SPLITS)

kernel_name = "hybrid_dilated_swa_dsa_block"


def rmsnorm(x, g):
    xf = x.astype(jnp.float32)
    y = xf * lax.rsqrt(jnp.mean(xf * xf, axis=-1, keepdims=True) + EPS)
    return (y * g.astype(jnp.float32)).astype(x.dtype)


def alibi_slopes():
    s = 2.0 ** (-8.0 * (np.arange(N_HEADS, dtype=np.float32) + 1.0) / N_HEADS)
    return jnp.asarray(s[0::2], jnp.float32), jnp.asarray(s[1::2], jnp.float32)


def dilated_branch(q, k, v, window, dilation, slopes):
    bsz, seq, nh, hd = q.shape
    n = seq // dilation
    nb = -(-n // BLOCK)
    n_pad = nb * BLOCK
    w_res = window // dilation
    assert w_res <= BLOCK
    g = bsz * dilation

    def to_res(a):
        a = a.reshape(bsz, n, dilation, nh, hd).transpose(0, 2, 3, 1, 4).reshape(g, nh, n, hd)
        return jnp.pad(a, ((0, 0), (0, 0), (0, n_pad - n), (0, 0)))

    qr, kr, vr = to_res(q), to_res(k), to_res(v)

    def band(a):
        cur = a.reshape(g, nh, nb, BLOCK, hd)
        prev = jnp.pad(a, ((0, 0), (0, 0), (BLOCK, 0), (0, 0)))[:, :, :n_pad].reshape(g, nh, nb, BLOCK, hd)
        return jnp.concatenate([prev, cur], axis=3)

    kb, vb = band(kr), band(vr)
    qb = qr.reshape(g, nh, nb, BLOCK, hd)
    s = jnp.einsum('ghnqd,ghnkd->ghnqk', qb, kb).astype(jnp.float32) * (hd ** -0.5)
    dist = np.arange(BLOCK)[:, None] - np.arange(2 * BLOCK)[None, :] + BLOCK
    key_pos = np.arange(nb)[:, None] * BLOCK - BLOCK + np.arange(2 * BLOCK)[None, :]
    mask = ((dist >= 0) & (dist <= w_res))[None] & (key_pos >= 0)[:, None, :]
    bias = -slopes[:, None, None] * jnp.asarray(dist * dilation, jnp.float32)[None]
    s = jnp.where(mask[None, None], s + bias[None, :, None], -jnp.inf)
    m = jnp.max(s, axis=-1, keepdims=True)
    p = jnp.exp(s - m)
    l = jnp.sum(p, axis=-1, keepdims=True)
    o = jnp.einsum('ghnqk,ghnkd->ghnqd', (p / l).astype(v.dtype), vb)
    lse = (m + jnp.log(l))[..., 0]
    o = o.reshape(g, nh, n_pad, hd)[:, :, :n].reshape(bsz, dilation, nh, n, hd)
    o = o.transpose(0, 3, 1, 2, 4).reshape(bsz, seq, nh, hd)
    lse = lse.reshape(g, nh, n_pad)[:, :, :n].reshape(bsz, dilation, nh, n)
    lse = lse.transpose(0, 3, 1, 2).reshape(bsz, seq, nh)
    return o, lse


def dilated_mixture(q, k, v, slopes):
    outs, lses = [], []
    for window, dilation in DILATED_CONFIGS:
        o, lse = dilated_branch(q, k, v, window, dilation, slopes)
        outs.append(o.astype(jnp.float32))
        lses.append(lse)
    alpha = jax.nn.softmax(jnp.stack(lses, 0), axis=0)
    out = jnp.sum(alpha[..., None] * jnp.stack(outs, 0), axis=0)
    return out.astype(q.dtype)


def sparse_attention(q_idx, k_idx, w_idx, q_lat, c_kv, slopes):
    bsz, seq = c_kv.shape[0], c_kv.shape[1]
    nb = seq // BLOCK
    topk = min(TOPK_MAX, seq // 4)
    s_pos = jnp.arange(seq)
    k_idx_f = k_idx.astype(jnp.float32)

    def block_fn(i):
        start = i * BLOCK
        qi = lax.dynamic_slice_in_dim(q_idx, start, BLOCK, axis=1).astype(jnp.float32)
        wi = lax.dynamic_slice_in_dim(w_idx, start, BLOCK, axis=1).astype(jnp.float32)
        ql = lax.dynamic_slice_in_dim(q_lat, start, BLOCK, axis=1)
        t_pos = start + jnp.arange(BLOCK)
        logits = jnp.einsum('bqhd,bkd->bqhk', qi, k_idx_f)
        score = jnp.einsum('bqh,bqhk->bqk', wi, jax.nn.relu(logits))
        score = jnp.where(s_pos[None, None, :] <= t_pos[None, :, None], score, -jnp.inf)
        _, idx = lax.top_k(score, topk)
        c_sel = jax.vmap(lambda cb, ib: cb[ib])(c_kv, idx)
        s = jnp.einsum('bqhr,bqkr->bhqk', ql, c_sel).astype(jnp.float32) * (HEAD_DIM ** -0.5)
        dist = (t_pos[None, :, None] - idx).astype(jnp.float32)
        s = s - slopes[None, :, None, None] * dist[:, None]
        s = jnp.where((idx <= t_pos[None, :, None])[:, None], s, -jnp.inf)
        p = jax.nn.softmax(s, axis=-1)
        return jnp.einsum('bhqk,bqkr->bqhr', p.astype(c_sel.dtype), c_sel)

    o = lax.map(block_fn, jnp.arange(nb))
    return o.transpose(1, 0, 2, 3, 4).reshape(bsz, seq, o.shape[3], o.shape[4])


def setup_inputs(seed: int = 0) -> dict:
    key = jax.random.key(seed)
    ks = jax.random.split(key, 16)
    f32 = jnp.float32
    nrm = lambda k, shape, scale: jax.random.normal(k, shape, f32) * scale
    return {
        "x": nrm(ks[0], (BATCH, SEQ, D_MODEL), 1.0),
        "c": nrm(ks[1], (BATCH, D_MODEL), 1.0),
        "w_ada": nrm(ks[2], (DEPTH, D_MODEL, 6 * D_MODEL), 0.5 * D_MODEL ** -0.5),
        "b_ada": nrm(ks[3], (DEPTH, 6 * D_MODEL), 0.01),
        "g_attn": 1.0 + nrm(ks[4], (DEPTH, D_MODEL), 0.02),
        "w_in": nrm(ks[5], (DEPTH, D_MODEL, D_IN), D_MODEL ** -0.5),
        "kv_norm_g": 1.0 + nrm(ks[6], (DEPTH, KV_RANK), 0.02),
        "w_uk": nrm(ks[7], (DEPTH, N_HEADS_B, HEAD_DIM, KV_RANK), HEAD_DIM ** -0.5),
        "w_uv": nrm(ks[8], (DEPTH, N_HEADS_B, KV_RANK, HEAD_DIM), KV_RANK ** -0.5),
        "w_out": nrm(ks[9], (DEPTH, D_MODEL, D_MODEL), D_MODEL ** -0.5),
        "g_ffn": 1.0 + nrm(ks[10], (DEPTH, D_MODEL), 0.02),
        "w_gu": nrm(ks[11], (DEPTH, D_MODEL, 2 * D_FF), D_MODEL ** -0.5),
        "w_down": nrm(ks[12], (DEPTH, D_FF, D_MODEL), D_FF ** -0.5),
        "g_final": 1.0 + nrm(ks[13], (D_MODEL,), 0.02),
    }


def reference(x, c, w_ada, b_ada, g_attn, w_in, kv_norm_g, w_uk, w_uv, w_out, g_ffn, w_gu, w_down, g_final):
    bsz, seq, _ = x.shape
    slopes_a, slopes_b = alibi_slopes()
    offsets = [int(o) for o in np.cumsum(SPLITS)[:-1]]
    c_act = jax.nn.silu(c)
    for l in range(DEPTH):
        mod = c_act @ w_ada[l] + b_ada[l]
        sh1, sc1, ga1, sh2, sc2, ga2 = [m[:, None, :] for m in jnp.split(mod, 6, axis=-1)]

        h = rmsnorm(x, g_attn[l]) * (1.0 + sc1) + sh1
        proj = h @ w_in[l]
        qa, ka, va, qb, ckv, qi, ki, wi = jnp.split(proj, offsets, axis=-1)
        shp_a = (bsz, seq, N_HEADS_A, HEAD_DIM)
        out_a = dilated_mixture(qa.reshape(shp_a), ka.reshape(shp_a), va.reshape(shp_a), slopes_a)
        ckv = rmsnorm(ckv, kv_norm_g[l])
        q_lat = jnp.einsum('bthd,hdr->bthr', qb.reshape(bsz, seq, N_HEADS_B, HEAD_DIM), w_uk[l])
        o_lat = sparse_attention(qi.reshape(bsz, seq, IDX_HEADS, IDX_DIM), ki, wi * IDX_SCALE,
                                 q_lat, ckv, slopes_b)
        out_b = jnp.einsum('bthr,hrd->bthd', o_lat, w_uv[l])
        mixed = jnp.concatenate([out_a.reshape(bsz, seq, -1), out_b.reshape(bsz, seq, -1)], axis=-1)
        x = x + ga1 * (mixed @ w_out[l])

        h2 = rmsnorm(x, g_ffn[l]) * (1.0 + sc2) + sh2
        gate, up = jnp.split(h2 @ w_gu[l], 2, axis=-1)
        x = x + ga2 * ((jax.nn.silu(gate) * up) @ w_down[l])
    return rmsnorm(x, g_final)
```

```python
import numpy as np
import concourse.bass as bass
import concourse.mybir as mybir
from concourse.bass_utils import run_bass_kernel_spmd

F32 = mybir.dt.float32
BF16 = mybir.dt.bfloat16
U8 = mybir.dt.uint8
AF = mybir.ActivationFunctionType
ALU = mybir.AluOpType
AX = mybir.AxisListType

D = 1024
T = 4096
NT = T // 128
DFF = 2816
NJ = DFF // 128
EPS = 1e-6
NEG = -30000.0
IDX_SCALE = 512.0 ** -0.5
TOPK = 256
NBIS = 13

COMPUTE = ("pe", "act", "dve", "pool")
CH = 16384
NDMA = 8


class Prog:
    def __init__(self, nc, es):
        self.nc = nc
        self.es = es
        self.engs = {"pe": nc.tensor, "act": nc.scalar, "dve": nc.vector, "pool": nc.gpsimd, "sp": nc.sync}
        self.ops = {e: [] for e in self.engs}
        self.cnt = {}
        self.sems = {}
        self.last_w = {}
        self.readers = {}
        self.seen = {e: {} for e in self.engs}
        self.dma_n = {e: 0 for e in self.engs}

    def _sem(self, name):
        if name not in self.sems:
            self.sems[name] = self.es.enter_context(self.nc.semaphore("s_" + name))
        return self.sems[name]

    def _tok_wait(self, tok):
        src, idx = tok
        if src[0] == "dma":
            return (self._sem("d_%s_%d" % (src[1], src[2])), 16 * (idx + 1))
        return (self._sem("%s_%d" % (src[0], idx // CH)), idx % CH + 1)

    def _need(self, eng, tok, waits):
        if tok is None:
            return
        src, idx = tok
        if self.seen[eng].get(src, -1) >= idx:
            return
        self.seen[eng][src] = idx
        waits.append(self._tok_wait(tok))

    def _deps(self, eng, reads, writes, is_dma=False):
        waits = []
        for k in reads:
            tok = self.last_w.get(k)
            if tok is not None and not (tok[0] == ("pe",) and eng == "pe"):
                self._need(eng, tok, waits)
        for k in writes:
            tok = self.last_w.get(k)
            if tok is not None and (is_dma or tok[0] != (eng,)):
                self._need(eng, tok, waits)
            for r in self.readers.get(k, ()):
                if is_dma or r[0] != (eng,):
                    self._need(eng, r, waits)
        return waits

    def op(self, eng, fn, reads=(), writes=()):
        waits = self._deps(eng, reads, writes)
        idx = self.cnt.get(eng, 0)
        self.cnt[eng] = idx + 1
        tok = ((eng,), idx)
        inc = (self._sem("%s_%d" % (eng, idx // CH)), 1)
        self.ops[eng].append((waits, fn, inc))
        for k in reads:
            self.readers.setdefault(k, []).append(tok)
        for k in writes:
            self.last_w[k] = tok
            self.readers[k] = []
        return tok

    def dma(self, q, out, in_, reads=(), writes=()):
        waits = self._deps(q, reads, writes, is_dma=True)
        n = self.dma_n[q]
        self.dma_n[q] = n + 1
        slot = n % NDMA
        idx = n // NDMA
        src = ("dma", q, slot)
        if idx > 0:
            self._need(q, (src, idx - 1), waits)
        tok = (src, idx)
        inc = (self._sem("d_%s_%d" % (q, slot)), 16)
        self.ops[q].append((waits, lambda e: e.dma_start(out=out, in_=in_), inc))
        for k in reads:
            self.readers.setdefault(k, []).append(tok)
        for k in writes:
            self.last_w[k] = tok
            self.readers[k] = []
        return tok

    def barrier(self):
        toks = []
        for e in COMPUTE:
            if self.cnt.get(e, 0) > 0:
                toks.append(((e,), self.cnt[e] - 1))
        for q in self.engs:
            n = self.dma_n[q]
            for slot in range(min(n, NDMA)):
                cntslot = (n - slot + NDMA - 1) // NDMA
                if cntslot > 0:
                    toks.append((("dma", q, slot), cntslot - 1))
        for e in self.engs:
            waits = []
            for tok in toks:
                self._need(e, tok, waits)
            if waits:
                self.ops[e].append((waits, None, None))
        self.last_w = {}
        self.readers = {}

    def emit(self):
        with self.nc.Block() as block:
            def mk(ename):
                def body(eng):
                    for waits, fn, inc in self.ops[ename]:
                        for s, v in waits:
                            eng.wait_ge(s, v)
                        if fn is not None:
                            ins = fn(eng)
                            ins.then_inc(inc[0], inc[1])
                return body
            block.tensor(mk("pe"))
            block.scalar(mk("act"))
            block.vector(mk("dve"))
            block.gpsimd(mk("pool"))
            block.sync(mk("sp"))


class Arena:
    def __init__(self, t, size):
        self.t = t
        self.size = size
        self.off = 0

    def alloc_at(self, off, shape, dtype):
        save = self.off
        self.off = off
        v = self.alloc(shape, dtype)
        end = self.off
        self.off = save
        return v, end

    def alloc(self, shape, dtype):
        esz = mybir.dt.size(dtype)
        n = 1
        for s in shape[1:]:
            n *= s
        nbytes = (n * esz + 31) // 32 * 32
        assert self.off + nbytes <= self.size, ("arena overflow", self.off, nbytes, self.size)
        v = self.t[0:128, self.off:self.off + nbytes]
        if dtype != U8:
            v = v.bitcast(dtype)
        v = v[:, 0:n]
        self.off += nbytes
        if len(shape) == 3:
            v = v.rearrange("p (a b) -> p a b", b=shape[2])
        elif len(shape) == 4:
            v = v.rearrange("p (a b c) -> p a b c", b=shape[2], c=shape[3])
        if shape[0] != 128:
            v = v[0:shape[0]]
        return v


def alibi_slopes():
    s = 2.0 ** (-8.0 * (np.arange(16, dtype=np.float32) + 1.0) / 16)
    return s[0::2].astype(np.float32), s[1::2].astype(np.float32)


def make_consts():
    sa, sb = alibi_slopes()
    c = {}
    c["ident"] = np.eye(128, dtype=np.float32)
    c["identrep"] = np.tile(np.eye(128, dtype=np.float32), (1, 4))
    ik = np.arange(128)[:, None]
    iq = np.arange(128)[None, :]
    bA = np.zeros((128, 3, 2, 8, 128), np.float32)
    for ci, dil in enumerate((1, 4, 16)):
        for h in range(8):
            dprev = iq - ik + 128
            bA[:, ci, 0, h, :] = np.where(dprev <= 128, -sa[h] * dil * dprev, NEG)
            dcur = iq - ik
            bA[:, ci, 1, h, :] = np.where(dcur >= 0, -sa[h] * dil * dcur, NEG)
    c["biasA"] = bA.reshape(128, -1)
    al = np.zeros((3, 32, 128), np.float32)
    for d in range(32):
        al[0, d, :] = d
        al[1, d, :] = 1.0
        al[2, d, :] = np.arange(128)
    c["alL"] = al.reshape(3, -1)
    ar = np.zeros((3, 8, 128), np.float32)
    for h in range(8):
        ar[0, h, :] = -1024.0 * sb[h]
        ar[1, h, :] = -8.0 * sb[h] * np.arange(128)
        ar[2, h, :] = 8.0 * sb[h]
    c["alR"] = ar.reshape(3, -1)
    cm = np.where(np.arange(128)[None, :] <= np.arange(128)[:, None], 0.0, -1e30).astype(np.float32)
    c["causal"] = cm
    c["pow2"] = np.tile((2.0 ** -np.arange(0, NBIS + 2, dtype=np.float32))[None, :], (128, 1)).astype(np.float32)
    return c


def pipeline(gens, depth):
    active = []
    nxt = 0
    while active or nxt < len(gens):
        while len(active) < depth and nxt < len(gens):
            active.append(gens[nxt])
            nxt += 1
        for g in list(active):
            try:
                next(g)
            except StopIteration:
                active.remove(g)


def interleave(*gw):
    gens = [[g, max(1, n), 0] for g, n in zip(gw[0::2], gw[1::2]) if g is not None]
    while gens:
        best = min(gens, key=lambda x: x[2] / x[1])
        try:
            next(best[0])
            best[2] += 1
        except StopIteration:
            gens.remove(best)


def phase2a(P, ar, psf, psh, qaT, kaT, V_d, pA_d, cst_d):
    biasA = ar.alloc([128, 6, 8, 128], F32)
    SsbA = [ar.alloc([128, 4, 512], F32) for _ in range(2)]
    PTA = [ar.alloc([128, 4, 512], BF16) for _ in range(2)]
    Ssb = [[[SsbA[b][:, w * 2 + p, :].rearrange("p (a c) -> p a c", c=128) for p in range(2)] for w in range(2)] for b in range(2)]
    PT = [[[PTA[b][:, w * 2 + p, :] for p in range(2)] for w in range(2)] for b in range(2)]
    Vb = [ar.alloc([128, 8, 65], BF16) for _ in range(3)]
    ob = [ar.alloc([128, 520], F32) for _ in range(2)]
    P.dma("sp", biasA.rearrange("p a h q -> p (a h q)"), cst_d["biasA"], writes=["biasA"])
    blocks = []
    for ci, dil in enumerate((1, 4, 16)):
        nb = T // dil // 128
        for r in range(dil):
            for n in range(nb):
                blocks.append((ci, dil, r, n))
    nblk = len(blocks)

    def tsl(dil, r, n):
        base = r + dil * 128 * n
        return slice(base, base + dil * 127 + 1, dil)

    for idx in range(nblk + 1):
        if idx < nblk:
            ci, dil, r, n = blocks[idx]
            bp = idx % 2
            vs = idx % 3
            P.dma("sp", Vb[vs].rearrange("p h d -> p (h d)"), V_d[tsl(dil, r, n), :], writes=["Vb%d" % vs])
            whichs = (1,) if n == 0 else (0, 1)
            for which in whichs:
                kn = n - 1 if which == 0 else n
                for par in range(2):
                    bank = which * 2 + par

                    def qk(e, dil=dil, r=r, n=n, kn=kn, par=par, bank=bank):
                        ins = None
                        p0 = par * 64
                        for hh in range(4):
                            ins = e.matmul(psf(bank)[:, hh * 128:(hh + 1) * 128],
                                           kaT[p0:p0 + 64, hh, tsl(dil, r, kn)], qaT[p0:p0 + 64, hh, tsl(dil, r, n)],
                                           start=True, stop=True)
                        return ins
                    P.op("pe", qk, writes=["ps%d" % bank])
                    sb = Ssb[bp][which][par]
                    P.op("dve", lambda e, sb=sb, bank=bank, ci=ci, which=which, par=par: e.scalar_tensor_tensor(
                        out=sb, in0=psf(bank).rearrange("p (a b) -> p a b", b=128), scalar=0.125, op0=ALU.mult,
                        in1=biasA[:, ci * 2 + which, par::2, :], op1=ALU.add),
                        reads=["ps%d" % bank, "biasA"], writes=["Ssb%d%d%d" % (bp, which, par)])
            w0 = whichs[0] * 2
            P.op("act", lambda e, bp=bp, w0=w0: e.activation(out=PTA[bp][:, w0:4, :], in_=SsbA[bp][:, w0:4, :], func=AF.Exp),
                 reads=["Ssb%d%d%d" % (bp, w, p) for w in whichs for p in range(2)], writes=["PT%d%d%d" % (bp, w, p) for w in whichs for p in range(2)])
        if idx >= 1:
            j = idx - 1
            ci, dil, r, n = blocks[j]
            bp = j % 2
            whichs = (1,) if n == 0 else (0, 1)
            oa, obk = (4, 5) if bp == 0 else (6, 7)

            def pv(e, bp=bp, j=j, whichs=whichs, oa=oa, obk=obk):
                ins = None
                for h in range(8):
                    par, hh = h % 2, h // 2
                    bank = oa if h < 4 else obk
                    col = (h % 4) * 65
                    for wi_, which in enumerate(whichs):
                        vsl = (j - 1) % 3 if which == 0 else j % 3
                        ins = e.matmul(psf(bank)[:, col:col + 65], PT[bp][which][par][:, hh * 128:(hh + 1) * 128],
                                       Vb[vsl][:, h, :], start=(wi_ == 0), stop=(wi_ == len(whichs) - 1))
                return ins
            rd = ["PT%d%d%d" % (bp, w, p) for w in whichs for p in range(2)] + ["Vb%d" % (j % 3)]
            if n > 0:
                rd.append("Vb%d" % ((j - 1) % 3))
            P.op("pe", pv, reads=rd, writes=["ps%d" % oa, "ps%d" % obk])
            o = ob[bp]
            P.op("act", lambda e, o=o, oa=oa: e.copy(out=o[:, 0:260], in_=psf(oa)[:, 0:260]), reads=["ps%d" % oa], writes=["ob%da" % bp])
            P.op("dve", lambda e, o=o, obk=obk: e.tensor_copy(out=o[:, 260:520], in_=psf(obk)[:, 0:260]), reads=["ps%d" % obk], writes=["ob%db" % bp])
            P.dma("sp", pA_d[ci, tsl(dil, r, n), :], o, reads=["ob%da" % bp, "ob%db" % bp], writes=["pA_d"])


def rstd_ops(P, ss, ms, rstd, nhalf, scale, keys):
    k_ss, k_ms, k_r = keys
    P.op("dve", lambda e: e.tensor_scalar(out=ms, in0=ss, scalar1=scale, scalar2=EPS, op0=ALU.mult, op1=ALU.add), reads=[k_ss], writes=[k_ms])
    P.op("pool", lambda e: e.tensor_tensor(out=rstd, in0=ms, in1=nhalf, op=ALU.pow), reads=[k_ms, "nhalf"], writes=[k_r])


def phase2b(P, ar, psf, psh, L):
    x_d, mix_d, pA_d, cst_d = L["x_d"], L["mix_d"], L["pA_d"], L["cst_d"]
    win_d, wuk_d, wuv_d = L["win_d"], L["wuk_d"], L["wuv_d"]
    kiT, ckv, ckvT, ident = L["kiT"], L["ckv"], L["ckvT"], L["ident"]
    SH1, GM1 = L["SH1"], L["GM1"]
    w2 = ar.alloc([128, 8, 1032], BF16)
    wuk = ar.alloc([128, 4, 128], BF16)
    wuv = ar.alloc([128, 8, 64], BF16)
    identrep = ar.alloc([128, 512], BF16)
    alL = ar.alloc([128, 32, 128], BF16)
    alR = ar.alloc([128, 1024], BF16)
    causal = ar.alloc([128, 128], F32)
    pow2 = ar.alloc([128, NBIS + 2], F32)
    nhalf = ar.alloc([128, 1], F32)
    xq = ar.alloc([128, D], F32)
    junk = ar.alloc([128, D], BF16)
    hb = ar.alloc([128, D], BF16)
    tmpf = ar.alloc([128, D], F32)
    hTq = ar.alloc([128, 8, 128], BF16)
    qbT = ar.alloc([128, 4, 128], BF16)
    qis = ar.alloc([128, 8, 64], BF16)
    qiT = ar.alloc([128, 8, 128], BF16)
    wis = ar.alloc([128, 8], F32)
    cmax = [ar.alloc([128, 8], F32) for _ in range(2)]
    dg = ar.alloc([128, 8, 128], BF16)
    rl = [ar.alloc([128, 512], BF16) for _ in range(4)]
    st = ar.alloc([128, 8], F32)
    stb = ar.alloc([128, 8], F32)
    wk = ar.alloc([128, NBIS + 2], F32)
    score = [ar.alloc([128, T], F32) for _ in range(2)]
    mb = [ar.alloc([128, T], BF16) for _ in range(2)]
    qlT = [ar.alloc([128, 8, 128], BF16) for _ in range(3)]
    thr = [ar.alloc([128, 1], F32) for _ in range(2)]
    PTA = [ar.alloc([128, 2, 512], BF16) for _ in range(2)]
    PT = [[PTA[b][:, h_, :] for h_ in range(2)] for b in range(2)]
    rec = ar.alloc([128, 8], F32)
    recA = ar.alloc([128, 8], F32)
    olat = ar.alloc([128, 8, 128], BF16)
    olatT = ar.alloc([128, 8, 128], BF16)
    mixed = [ar.alloc([128, D], BF16) for _ in range(2)]
    pAt = ar.alloc([128, 3, 520], F32)
    tot = ar.alloc([128, 8, 65], F32)

    win_v = win_d.rearrange("(c p) n -> p c n", p=128)
    P.dma("pool", w2[:, :, 0:512], win_v[:, :, 1536:2048], writes=["w2"])
    P.dma("pool", w2[:, :, 512:1024], win_v[:, :, 2176:2688], writes=["w2"])
    P.dma("pool", w2[:, :, 1024:1032], win_v[:, :, 2752:2760], writes=["w2"])
    P.dma("pool", wuk, wuk_d.rearrange("(c two) d r -> (two d) c r", two=2), writes=["wuk"])
    P.dma("pool", wuv, wuv_d.rearrange("h r d -> r h d"), writes=["wuv"])
    P.dma("pool", identrep, cst_d["identrep"], writes=["identrep"])
    P.op("pool", lambda e: e.memset(alL, 0.0), writes=["alL"])
    P.op("pool", lambda e: e.memset(alR, 0.0), writes=["alR"])
    P.dma("pool", alL[0:3].rearrange("p a b -> p (a b)"), cst_d["alL"], writes=["alL"])
    P.dma("pool", alR[0:3], cst_d["alR"], writes=["alR"])
    P.dma("sp", causal, cst_d["causal"], writes=["causal"])
    P.dma("sp", pow2, cst_d["pow2"], writes=["pow2"])
    P.op("pool", lambda e: e.memset(nhalf, -0.5), writes=["nhalf"])
    P.op("pool", lambda e: e.memset(qiT[64:128], 0.0), writes=["qiT_z"])
    x_v = x_d.rearrange("(n p) d -> n p d", p=128)
    mix_v = mix_d.rearrange("(n p) d -> n p d", p=128)
    assert len(L["precast"]) <= NT + 2

    def stageA1(i):
        sp_ = i % 2
        q3 = i % 3
        sc = score[sp_]
        ksc = "score%d" % sp_
        N = 128 * (i + 1)
        P.dma("sp", xq, x_v[i], writes=["xq"])
        P.op("act", lambda e: e.activation(out=junk, in_=xq, func=AF.Square, accum_out=st[:, 0:1]), reads=["xq"], writes=["junk", "st0"])
        rstd_ops(P, st[:, 0:1], st[:, 1:2], st[:, 2:3], nhalf, 1.0 / D, ("st0", "st1", "st2"))
        P.op("dve", lambda e: e.scalar_tensor_tensor(out=tmpf, in0=xq, scalar=st[:, 2:3], op0=ALU.mult, in1=GM1, op1=ALU.mult),
             reads=["xq", "st2"], writes=["tmpf"])
        yield
        P.op("pool", lambda e: e.tensor_tensor(out=hb, in0=tmpf, in1=SH1, op=ALU.add), reads=["tmpf"], writes=["hb"])

        def tr(e):
            ins = None
            for c in range(8):
                ins = e.transpose(psh(0)[:, c * 128:(c + 1) * 128], hb[:, c * 128:(c + 1) * 128], ident)
            return ins
        P.op("pe", tr, reads=["hb"], writes=["ps0"])
        P.op("act", lambda e: e.copy(out=hTq, in_=psh(0).rearrange("p (c t) -> p c t", t=128)), reads=["ps0"], writes=["hTq"])
        yield

        def mqb(e):
            ins = None
            for cc in range(4):
                for kc in range(8):
                    ins = e.matmul(psf(1)[:, cc * 128:(cc + 1) * 128], w2[:, kc, cc * 128:(cc + 1) * 128], hTq[:, kc, :],
                                   start=(kc == 0), stop=(kc == 7))
            return ins
        P.op("pe", mqb, reads=["hTq", "w2"], writes=["ps1"])
        P.op("act", lambda e: e.copy(out=qbT, in_=psf(1).rearrange("p (c t) -> p c t", t=128)), reads=["ps1"], writes=["qbT"])

        def mqi(e):
            ins = None
            for kc in range(8):
                ins = e.matmul(psf(2), hTq[:, kc, :], w2[:, kc, 512:1024], start=(kc == 0), stop=(kc == 7))
            return ins
        P.op("pe", mqi, reads=["hTq", "w2"], writes=["ps2"])

        def mwi(e):
            ins = None
            for kc in range(8):
                ins = e.matmul(psf(0)[:, 0:8], hTq[:, kc, :], w2[:, kc, 1024:1032], start=(kc == 0), stop=(kc == 7))
            return ins
        P.op("pe", mwi, reads=["hTq", "w2"], writes=["ps0"])
        yield
        P.op("act", lambda e: e.activation(out=wis, in_=psf(0)[:, 0:8], func=AF.Copy, scale=IDX_SCALE), reads=["ps0"], writes=["wis"])
        P.op("dve", lambda e: e.tensor_tensor(out=dg, in0=ident.unsqueeze(1).broadcast_to([128, 8, 128]),
                                              in1=wis.unsqueeze(2).broadcast_to([128, 8, 128]), op=ALU.mult),
             reads=["wis"], writes=["dg"])
        P.op("act", lambda e: e.copy(out=qis, in_=psf(2).rearrange("p (h d) -> p h d", d=64)), reads=["ps2"], writes=["qis"])

        def tq(e):
            ins = None
            for s_ in range(8):
                ins = e.transpose(psh(2)[0:64, s_ * 128:(s_ + 1) * 128], qis[:, s_, :], ident)
            return ins
        P.op("pe", tq, reads=["qis"], writes=["ps2"])
        P.op("act", lambda e: e.copy(out=qiT[0:64], in_=psh(2)[0:64, :].rearrange("p (c t) -> p c t", t=128)), reads=["ps2"], writes=["qiT"])
        yield

        def mql(e):
            ins = None
            for h in range(8):
                p0 = (h % 2) * 64
                ins = e.matmul(psf(1 + h % 2)[:, (h // 2) * 128:(h // 2 + 1) * 128], wuk[p0:p0 + 64, h // 2, :], qbT[p0:p0 + 64, h // 2, :],
                               start=True, stop=True)
            return ins
        P.op("pe", mql, reads=["qbT", "wuk"], writes=["ps1", "ps2"])
        P.op("act", lambda e: e.copy(out=qlT[q3][:, 0::2, :], in_=psf(1).rearrange("p (c t) -> p c t", t=128)), reads=["ps1"], writes=["qlTa%d" % q3])
        P.op("dve", lambda e: e.tensor_copy(out=qlT[q3][:, 1::2, :], in_=psf(2).rearrange("p (c t) -> p c t", t=128)), reads=["ps2"], writes=["qlTb%d" % q3])
        yield
        nch = (N + 511) // 512
        for c in range(nch):
            W = min(512, N - 512 * c)
            ksl = slice(512 * c, 512 * c + W)

            def emit_L(h, ksl=ksl, W=W):
                b = 1 + h % 2
                P.op("pe", lambda e, h=h, b=b: e.matmul(psf(b)[:, 0:W], qiT[:, h, :], kiT[:, ksl], start=True, stop=True),
                     reads=["qiT"], writes=["ps%d" % b])
                rs = rl[h % 4]
                P.op("act", lambda e, b=b, rs=rs: e.activation(out=rs[:, 0:W], in_=psf(b)[:, 0:W], func=AF.Relu),
                     reads=["ps%d" % b], writes=["rl%d" % (h % 4)])

            def emit_D(h, W=W):
                rs = rl[h % 4]
                P.op("pe", lambda e, h=h, rs=rs: e.matmul(psf(0)[:, 0:W], dg[:, h, :], rs[:, 0:W], start=(h == 0), stop=(h == 7)),
                     reads=["dg", "rl%d" % (h % 4)], writes=["ps0"])
            emit_L(0)
            emit_L(1)
            for h in range(8):
                if h + 2 < 8:
                    emit_L(h + 2)
                emit_D(h)
                if h % 2 == 1:
                    yield
            P.op("dve", lambda e, ksl=ksl, W=W, c=c: e.tensor_scalar(out=sc[:, ksl], in0=psf(0)[:, 0:W], scalar1=1.0, scalar2=None, op0=ALU.mult, op1=ALU.max,
                                                                       accum_out=cmax[sp_][:, c:c + 1]),
                 reads=["ps0"], writes=[ksc, "cmax%d" % sp_])
            yield

    def stageA2(i):
        sp_ = i % 2
        par = i % 2
        sc = score[sp_]
        ksc = "score%d" % sp_
        N = 128 * (i + 1)
        nch = (N + 511) // 512
        if i >= 2:
            P.op("dve", lambda e: e.tensor_reduce(out=stb[:, 0:1], in_=cmax[sp_][:, 0:nch], axis=AX.X, op=ALU.max), reads=["cmax%d" % sp_], writes=["rmax"])
            P.op("dve", lambda e: e.tensor_reduce(out=stb[:, 1:2], in_=sc[:, 0:N], axis=AX.X, op=ALU.min), reads=[ksc], writes=["rmin"])
            yield
        P.op("dve", lambda e: e.tensor_tensor(out=sc[:, N - 128:N], in0=sc[:, N - 128:N], in1=causal, op=ALU.add),
             reads=[ksc, "causal"], writes=[ksc])
        if i >= 2:
            P.op("dve", lambda e: e.tensor_tensor(out=stb[:, 2:3], in0=stb[:, 0:1], in1=stb[:, 1:2], op=ALU.subtract), reads=["rmax", "rmin"], writes=["R"])
            P.op("dve", lambda e: e.tensor_scalar(out=wk, in0=pow2, scalar1=stb[:, 2:3], scalar2=None, op0=ALU.mult), reads=["R", "pow2"], writes=["wk"])
            P.op("dve", lambda e: e.tensor_tensor(out=stb[:, 3:4], in0=wk[:, 1:2], in1=stb[:, 1:2], op=ALU.add), reads=["wk", "rmin"], writes=["mid"])
            for k in range(1, NBIS + 1):
                P.op("dve", lambda e: e.tensor_scalar(out=mb[par][:, 0:N], in0=sc[:, 0:N], scalar1=stb[:, 3:4], scalar2=None, op0=ALU.is_ge, op1=ALU.add,
                                                       accum_out=stb[:, 4:5]),
                     reads=[ksc, "mid"], writes=["mb%d" % par, "cnt"])
                last = (k == NBIS)
                P.op("dve", lambda e, last=last: e.tensor_scalar(out=stb[:, 5:6], in0=stb[:, 4:5], scalar1=TOPK - 0.5, scalar2=(1.0 if last else 0.5),
                                                                  op0=ALU.is_ge, op1=ALU.subtract),
                     reads=["cnt"], writes=["tq"])
                dst = thr[par] if last else stb[:, 3:4]
                P.op("dve", lambda e, k=k, dst=dst: e.scalar_tensor_tensor(out=dst, in0=stb[:, 5:6], scalar=wk[:, k:k + 1], op0=ALU.mult, in1=stb[:, 3:4], op1=ALU.add),
                     reads=["tq", "wk", "mid"], writes=["thr%d" % par if last else "mid"])
                yield
        else:
            P.op("dve", lambda e: e.memset(thr[par], -1e29), writes=["thr%d" % par])
        P.op("dve", lambda e: e.tensor_scalar(out=mb[par][:, 0:N], in0=sc[:, 0:N], scalar1=thr[par], scalar2=NEG, op0=ALU.is_lt, op1=ALU.mult),
             reads=[ksc, "thr%d" % par], writes=["mb%d" % par])
        yield

    def stageB(i):
        par = i % 2
        q3 = i % 3
        mx = mixed[i % 2]
        kmx = "mixed%d" % (i % 2)
        OB = [(5 + h // 3, (h % 3) * 129) for h in range(8)]
        P.dma("sp", pAt, pA_d[:, i * 128:(i + 1) * 128, :].rearrange("c p f -> p c f"), writes=["pAt"])
        totf = tot.rearrange("p h d -> p (h d)")
        P.op("pool", lambda e: e.tensor_tensor(out=totf, in0=pAt[:, 0, :], in1=pAt[:, 1, :], op=ALU.add), reads=["pAt"], writes=["tot"])
        P.op("pool", lambda e: e.tensor_tensor(out=totf, in0=totf, in1=pAt[:, 2, :], op=ALU.add), reads=["pAt", "tot"], writes=["tot"])

        def emit_pv(j):
            def pv(e, j=j):
                ins = None
                for h in range(8):
                    bank, col = OB[h]
                    ins = e.matmul(psf(bank)[:, col:col + 129], PT[j % 2][h // 4][:, (h % 4) * 128:(h % 4 + 1) * 128], ckv[:, j, :],
                                   start=(j == 0 and h % 3 == 0), stop=(j == i), skip_group_check=True)
                return ins
            P.op("pe", pv, reads=["PT%d0" % (j % 2), "PT%d1" % (j % 2)], writes=["ps5", "ps6", "ps7"])
        for j in range(i + 1):
            for half in range(2):
                def qk(e, j=j, half=half):
                    e.matmul(psf(3 + half), ckvT[:, j * 128:(j + 1) * 128], qlT[q3][:, half * 4:(half + 1) * 4, :], start=True, stop=False)
                    e.matmul(psf(3 + half), mb[par][:, j * 128:(j + 1) * 128], identrep, start=False, stop=False)
                    return e.matmul(psf(3 + half), alL[:, i - j, :], alR[:, half * 512:(half + 1) * 512], start=False, stop=True)
                P.op("pe", qk, reads=["qlTa%d" % q3, "qlTb%d" % q3, "mb%d" % par, "identrep", "alL", "alR"], writes=["ps%d" % (3 + half)])
            P.op("act", lambda e, j=j: e.activation(out=PTA[j % 2].rearrange("p a b -> p (a b)"), in_=psf(3, 2), func=AF.Exp, scale=0.125),
                 reads=["ps3", "ps4"], writes=["PT%d0" % (j % 2), "PT%d1" % (j % 2)])
            if j >= 1:
                emit_pv(j - 1)
            if j == 1:
                P.op("dve", lambda e: e.reciprocal(out=recA, in_=tot[:, :, 64]), reads=["tot"], writes=["recA"])
                P.op("dve", lambda e: e.tensor_tensor(out=mx[:, 0:512].rearrange("p (h d) -> p h d", d=64), in0=tot[:, :, 0:64],
                                                      in1=recA.unsqueeze(2).broadcast_to([128, 8, 64]), op=ALU.mult),
                     reads=["tot", "recA"], writes=[kmx + "a"])
            yield
        if i == 0:
            P.op("dve", lambda e: e.reciprocal(out=recA, in_=tot[:, :, 64]), reads=["tot"], writes=["recA"])
            P.op("dve", lambda e: e.tensor_tensor(out=mx[:, 0:512].rearrange("p (h d) -> p h d", d=64), in0=tot[:, :, 0:64],
                                                  in1=recA.unsqueeze(2).broadcast_to([128, 8, 64]), op=ALU.mult),
                 reads=["tot", "recA"], writes=[kmx + "a"])
        emit_pv(i)
        yield
        for b in range(3):
            n = 3 if b < 2 else 2
            ov = psf(5 + b)[:, 0:n * 129].rearrange("p (h c) -> p h c", c=129)
            P.op("dve", lambda e, b=b, n=n, ov=ov: e.tensor_scalar(out=rec[:, 3 * b:3 * b + n], in0=ov[:, :, 128], scalar1=1e-30, scalar2=None, op0=ALU.max),
                 reads=["ps%d" % (5 + b)], writes=["rec%d" % b])
            P.op("dve", lambda e, b=b, n=n: e.reciprocal(out=rec[:, 3 * b:3 * b + n], in_=rec[:, 3 * b:3 * b + n]), reads=["rec%d" % b], writes=["rec%d" % b])
            P.op("dve", lambda e, b=b, n=n, ov=ov: e.tensor_tensor(out=olat[:, 3 * b:3 * b + n, :], in0=ov[:, :, 0:128],
                                                                    in1=rec[:, 3 * b:3 * b + n].unsqueeze(2).broadcast_to([128, n, 128]), op=ALU.mult),
                 reads=["ps%d" % (5 + b), "rec%d" % b], writes=["olat%d" % b])
        yield

        def tro(e):
            ins = None
            for h in range(8):
                ins = e.transpose(psh(3)[:, h * 128:(h + 1) * 128], olat[:, h, :], ident)
            return ins
        P.op("pe", tro, reads=["olat0", "olat1", "olat2"], writes=["ps3"])
        yield
        P.op("act", lambda e: e.copy(out=olatT, in_=psh(3).rearrange("p (c t) -> p c t", t=128)), reads=["ps3"], writes=["olatT"])
        yield

        def mob(e):
            ins = None
            for h in range(8):
                ins = e.matmul(psf(4)[:, h * 64:(h + 1) * 64], olatT[:, h, :], wuv[:, h, :], start=True, stop=True)
            return ins
        P.op("pe", mob, reads=["olatT", "wuv"], writes=["ps4"])
        yield
        P.op("act", lambda e: e.copy(out=mx[:, 512:1024], in_=psf(4)), reads=["ps4"], writes=[kmx + "b"])
        P.dma("pool", mix_v[i], mx, reads=[kmx + "a", kmx + "b"], writes=["mix_d"])
        yield

    precast = list(L["precast"])
    wstage = [ar.alloc([128, 8, 704], BF16) for _ in range(2)]
    npc = 0
    pending = None
    for s_ in range(NT + 2):
        if pending is not None:
            P.dma("sp", pending[0], pending[1], reads=[pending[2]], writes=[pending[3]])
            pending = None
        if precast:
            o_, i_, k_ = precast.pop(0)
            stg = wstage[npc % 2]
            if k_.startswith("wdns"):
                stg = stg.rearrange("p a b -> p (a b)")[:, 0:2 * D].rearrange("p (a b) -> p a b", b=D)
            elif k_.startswith("wouts"):
                stg = stg[:, :, 0:512]
            P.dma("pool", stg, i_, writes=["wstage%d" % (npc % 2)])
            pending = (o_, stg, "wstage%d" % (npc % 2), k_)
            npc += 1
        g1 = stageA1(s_) if s_ < NT else None
        g2 = stageA2(s_ - 1) if 1 <= s_ <= NT else None
        g3 = stageB(s_ - 2) if 2 <= s_ <= NT + 1 else None
        n1 = 5 + ((s_ + 4) // 4) * 5
        n2 = NBIS + 3
        n3 = s_ + 5
        interleave(g1, n1, g2, n2, g3, n3)
    if pending is not None:
        P.dma("sp", pending[0], pending[1], reads=[pending[2]], writes=[pending[3]])


def build(debug=False, phases=(0, 1, 2, 3)):
    from contextlib import ExitStack
    nc = bass.Bass("TRN2", target_bir_lowering=False)
    es = ExitStack()

    def din(name, shape):
        return nc.dram_tensor(name, shape, F32, kind="ExternalInput").ap()

    x_d = din("x", [T, D])
    c_d = din("c", [128, 8])
    wada_d = din("w_ada", [D, 6 * D])
    bada_d = din("b_ada", [1, 6 * D])
    gattn_d = din("g_attn", [1, D])
    win_d = din("w_in", [D, 2760])
    kvg_d = din("kv_norm_g", [1, 128])
    wuk_d = din("w_uk", [8, 64, 128])
    wuv_d = din("w_uv", [8, 128, 64])
    wout_d = din("w_out", [D, D])
    gffn_d = din("g_ffn", [1, D])
    wgu_d = din("w_gu", [D, 2 * DFF])
    wdown_d = din("w_down", [DFF, D])
    gfin_d = din("g_final", [1, D])
    consts = make_consts()
    cst_d = {k: din("k_" + k, list(v.shape)) for k, v in consts.items()}
    out_d = nc.dram_tensor("out", [T, D], F32, kind="ExternalOutput").ap()
    V_d = nc.dram_tensor("V_scr", [T, 520], BF16, kind="Internal").ap()
    pA_d = nc.dram_tensor("pA_scr", [3, T, 520], F32, kind="Internal").ap()
    x1_d = nc.dram_tensor("x1_scr", [T, D], F32, kind="Internal").ap()
    mix_d = nc.dram_tensor("mix_scr", [T, D], BF16, kind="Internal").ap()
    wgu_s = nc.dram_tensor("wgu_scr", [128, 8, 2 * DFF], BF16, kind="Internal").ap()
    wdn_s = nc.dram_tensor("wdn_scr", [128, NJ, D], BF16, kind="Internal").ap()
    WP = 704
    wgu_v = wgu_d.rearrange("(c p) n -> p c n", p=128)
    wdn_v = wdown_d.rearrange("(c p) n -> p c n", p=128)
    precast = []
    for pc in range(DFF // WP):
        for gu in range(2):
            c0 = gu * DFF + pc * WP
            precast.append((wgu_s[:, :, c0:c0 + WP], wgu_v[:, :, c0:c0 + WP], "wgus%d_%d" % (gu, pc)))
    for jp in range(0, NJ, 2):
        precast.append((wdn_s[:, jp:jp + 2, :], wdn_v[:, jp:jp + 2, :], "wdns%d" % (jp // 2)))
    wout_s = nc.dram_tensor("wout_scr", [128, 8, D], BF16, kind="Internal").ap()
    wout_v = wout_d.rearrange("(c p) n -> p c n", p=128)
    precast.insert(0, (wout_s[:, :, 512:1024], wout_v[:, :, 512:1024], "wouts1"))
    precast.insert(0, (wout_s[:, :, 0:512], wout_v[:, :, 0:512], "wouts0"))
    dbg = {}
    if debug:
        dbg["pA"] = nc.dram_tensor("dbg_pA", [3, T, 520], F32, kind="ExternalOutput").ap()
        dbg["x1"] = nc.dram_tensor("dbg_x1", [T, D], F32, kind="ExternalOutput").ap()
        pA_d = dbg["pA"]
        x1_d = dbg["x1"]
        dbg["modB"] = nc.dram_tensor("dbg_modB", [128, 6 * D], F32, kind="ExternalOutput").ap()
        dbg["kaT"] = nc.dram_tensor("dbg_kaT", [128, 4 * T], BF16, kind="ExternalOutput").ap()
        dbg["ckv"] = nc.dram_tensor("dbg_ckv", [128, NT * 129], BF16, kind="ExternalOutput").ap()
        dbg["ckvT"] = nc.dram_tensor("dbg_ckvT", [128, T], BF16, kind="ExternalOutput").ap()

    ASZ = 207 * 1024
    arena_t = es.enter_context(nc.sbuf_tensor("arena", [128, ASZ], U8))
    ar = Arena(arena_t, ASZ)
    ps_all = es.enter_context(nc.psum_tensor("ps_all", [128, 8 * 512], F32))
    P = Prog(nc, es)

    def psf(i, n=1):
        return ps_all[:, i * 512:(i + n) * 512]

    def psh(i):
        return ps_all[:, i * 512:(i + 1) * 512].bitcast(BF16)

    modB2 = ar.alloc([128, 4 * D], F32)
    gfB = ar.alloc([128, D], F32)
    ident = ar.alloc([128, 128], BF16)
    p3_off = ar.off
    modB1 = ar.alloc([128, 2 * D], F32)
    SH1, GM1 = [modB1[:, i * D:(i + 1) * D] for i in range(2)]
    SH2, GM2, GA2, GA1 = [modB2[:, i * D:(i + 1) * D] for i in range(4)]
    persist_off = ar.off

    def modcol(j):
        if j < 4:
            return modB1[:, j * 512:(j + 1) * 512]
        if j < 6:
            return modB2[:, 3 * D + (j - 4) * 512:3 * D + (j - 3) * 512]
        return modB2[:, (j - 6) * 512:(j - 5) * 512]

    P.dma("pool", ident, cst_d["ident"], writes=["ident"])
    P.dma("sp", gfB, gfin_d.broadcast_to([128, D]), writes=["gfB"])
    W1C = 1728
    W1_OFF = ASZ - 28 * 1024
    w1, _ = ar.alloc_at(W1_OFF, [128, 8, W1C], BF16)
    if 1 in phases:
        win_v1 = win_d.rearrange("(c p) n -> p c n", p=128)
        for (d0, s0, n) in ((0, 0, 768), (768, 768, 768), (1536, 2048, 128), (1664, 2688, 64)):
            P.dma("pool", w1[:, :, d0:d0 + n], win_v1[:, :, s0:s0 + n], writes=["w1_%d" % d0])

    if 0 in phases:
        baB = ar.alloc([128, 6 * D], F32)
        g1B = ar.alloc([128, D], F32)
        g2B = ar.alloc([128, D], F32)
        cT = ar.alloc([128, 8], F32)
        ca = ar.alloc([128, 8], F32)
        CA = ar.alloc([128, 8, 128], F32)
        wa = [ar.alloc([128, 8, 512], F32) for _ in range(4)]
        P.dma("sp", cT, c_d, writes=["cT"])
        P.dma("sp", baB, bada_d.broadcast_to([128, 6 * D]), writes=["baB"])
        P.dma("sp", g1B, gattn_d.broadcast_to([128, D]), writes=["g1B"])
        P.dma("sp", g2B, gffn_d.broadcast_to([128, D]), writes=["g2B"])
        P.op("act", lambda e: e.activation(out=ca, in_=cT, func=AF.Silu), reads=["cT"], writes=["ca"])
        P.op("dve", lambda e: e.tensor_copy(out=CA, in_=ca.unsqueeze(2).broadcast_to([128, 8, 128])),
             reads=["ca"], writes=["CA"])
        wada_v = wada_d.rearrange("(c p) n -> p c n", p=128)
        for j in range(12):
            wt = wa[j % 4]
            P.dma("sp", wt, wada_v[:, :, j * 512:(j + 1) * 512], writes=["wa%d" % (j % 4)])
            pb = j % 2

            def mm(e, wt=wt, pb=pb):
                ins = None
                for kc in range(8):
                    ins = e.matmul(psf(pb), CA[:, kc, :], wt[:, kc, :], start=(kc == 0), stop=(kc == 7))
                return ins
            P.op("pe", mm, reads=["CA", "wa%d" % (j % 4)], writes=["ps%d" % pb])
            P.op("dve", lambda e, j=j, pb=pb: e.tensor_tensor(out=modcol(j), in0=psf(pb),
                                                               in1=baB[:, j * 512:(j + 1) * 512], op=ALU.add),
                 reads=["ps%d" % pb, "baB"], writes=["modB"])
        P.op("dve", lambda e: e.scalar_tensor_tensor(out=GM1, in0=GM1, scalar=1.0, op0=ALU.add, in1=g1B, op1=ALU.mult),
             reads=["modB", "g1B"], writes=["modB"])
        P.op("dve", lambda e: e.scalar_tensor_tensor(out=GM2, in0=GM2, scalar=1.0, op0=ALU.add, in1=g2B, op1=ALU.mult),
             reads=["modB", "g2B"], writes=["modB"])
        assert ar.off <= W1_OFF, ("phase-0 buffers overlap w1", ar.off, W1_OFF)
        if debug:
            P.dma("sp", dbg["modB"][:, 0:2 * D], modB1, reads=["modB"], writes=["dbg_modB"])
            P.dma("sp", dbg["modB"][:, 2 * D:3 * D], GA1, reads=["modB"], writes=["dbg_modB3"])
            P.dma("sp", dbg["modB"][:, 3 * D:6 * D], modB2[:, 0:3 * D], reads=["modB"], writes=["dbg_modB2"])
        P.barrier()
    ar.off = persist_off

    W1C = 1728
    kiT = ar.alloc([128, T], BF16)
    ckv = ar.alloc([128, NT, 129], BF16)
    ckvT = ar.alloc([128, T], BF16)
    p2b_off = ar.off
    qaT = ar.alloc([128, 4, T], BF16)
    kaT = ar.alloc([128, 4, T], BF16)
    p2a_off = ar.off
    if 1 in phases:
        kvgB = ar.alloc([128, 128], F32)
        RING = 3
        xt = [ar.alloc([128, D], F32) for _ in range(RING)]
        junk = ar.alloc([128, D], BF16)
        tmpf = [ar.alloc([128, D], F32) for _ in range(RING)]
        hb = [ar.alloc([128, D], BF16) for _ in range(RING)]
        hT = [ar.alloc([128, 8, 512], BF16) for _ in range(2)]
        vt = [ar.alloc([128, 8, 65], BF16) for _ in range(RING)]
        stR = [ar.alloc([128, 8], F32) for _ in range(RING)]
        nh1 = ar.alloc([128, 1], F32)
        assert ar.off <= W1_OFF, ("phase-1 buffers overlap w1", ar.off, W1_OFF)
        P.dma("sp", kvgB, kvg_d.broadcast_to([128, 128]), writes=["kvgB"])
        P.op("pool", lambda e: e.memset(nh1, -0.5), writes=["nhalf"])
        P.op("pool", lambda e: e.memset(ckv[:, :, 128:129], 1.0), writes=["ckv_ones"])
        P.op("pool", lambda e: e.memset(kiT[64:128, :], 0.0), writes=["kiT_z"])
        for i in range(RING):
            P.op("pool", lambda e, i=i: e.memset(vt[i][:, :, 64:65], 1.0), writes=["vt%d" % i])
        x_v = x_d.rearrange("(n p) d -> n p d", p=128)
        V_v = V_d.rearrange("(n p) c -> n p c", p=128)

        def tile1(t, part):
            g, tl = t // 4, t % 4
            hTg = hT[g % 2]
            kh = "hT%d" % (g % 2)
            s = t % RING
            st = stR[s]
            ks = "st%d_" % s
            pt_ = 0 if t % 2 == 0 else 7
            if part == 2:
                yield from tile1y(t, g, tl, hTg, kh, s, st, ks)
                return
            if part == 1:
                yield from tile1x2(t, tl, hTg, kh, s, pt_)
                return
            P.dma("sp", xt[s], x_v[t], writes=["xt%d" % s])
            P.op("act", lambda e: e.activation(out=junk, in_=xt[s], func=AF.Square, accum_out=st[:, 0:1]),
                 reads=["xt%d" % s], writes=["junk", ks + "0"])
            yield
            rstd_ops(P, st[:, 0:1], st[:, 1:2], st[:, 2:3], nh1, 1.0 / D, (ks + "0", ks + "1", ks + "2"))
            yield
            P.op("dve", lambda e: e.scalar_tensor_tensor(out=tmpf[s], in0=xt[s], scalar=st[:, 2:3], op0=ALU.mult, in1=GM1, op1=ALU.mult),
                 reads=["xt%d" % s, ks + "2", "modB"], writes=["tmpf%d" % s])
            yield

        def tile1x2(t, tl, hTg, kh, s, pt_):
            P.op("pool", lambda e: e.tensor_tensor(out=hb[s], in0=tmpf[s], in1=SH1, op=ALU.add),
                 reads=["tmpf%d" % s, "modB"], writes=["hb%d" % s])
            yield

            def tr(e):
                ins = None
                for c in range(8):
                    ins = e.transpose(psh(pt_)[:, c * 128:(c + 1) * 128], hb[s][:, c * 128:(c + 1) * 128], ident)
                return ins
            P.op("pe", tr, reads=["hb%d" % s, "ident"], writes=["ps%d" % pt_])
            yield
            P.op("act", lambda e: e.copy(out=hTg[:, :, tl * 128:(tl + 1) * 128], in_=psh(pt_).rearrange("p (c t) -> p c t", t=128)),
                 reads=["ps%d" % pt_], writes=[kh])
            yield

        def tile1y(t, g, tl, hTg, kh, s, st, ks):
            def mmv(e):
                ins = None
                for kc in range(8):
                    ins = e.matmul(psf(1), hTg[:, kc, tl * 128:(tl + 1) * 128], w1[:, kc, 1024:1536], start=(kc == 0), stop=(kc == 7))
                return ins
            P.op("pe", mmv, reads=[kh, "w1"], writes=["ps1"])

            def mmc(e):
                ins = None
                for kc in range(8):
                    ins = e.matmul(psf(2)[:, 0:128], hTg[:, kc, tl * 128:(tl + 1) * 128], w1[:, kc, 1536:1664], start=(kc == 0), stop=(kc == 7))
                return ins
            P.op("pe", mmc, reads=[kh, "w1"], writes=["ps2"])
            yield
            P.op("dve", lambda e: e.tensor_copy(out=vt[s][:, :, 0:64], in_=psf(1).rearrange("p (h d) -> p h d", d=64)),
                 reads=["ps1"], writes=["vt%d" % s])
            P.dma("sp", V_v[t], vt[s].rearrange("p h d -> p (h d)"), reads=["vt%d" % s], writes=["V_d"])
            P.op("act", lambda e: e.activation(out=junk[:, 0:128], in_=psf(2)[:, 0:128], func=AF.Square, accum_out=st[:, 3:4]),
                 reads=["ps2"], writes=["junk", ks + "3"])
            yield
            rstd_ops(P, st[:, 3:4], st[:, 4:5], st[:, 5:6], nh1, 1.0 / 128, (ks + "3", ks + "4", ks + "5"))
            yield
            P.op("dve", lambda e: e.scalar_tensor_tensor(out=ckv[:, t, 0:128], in0=psf(2)[:, 0:128], scalar=st[:, 5:6],
                                                         op0=ALU.mult, in1=kvgB, op1=ALU.mult),
                 reads=["ps2", ks + "5", "kvgB"], writes=["ckv%d" % t])
            yield
            P.op("pe", lambda e: e.transpose(psh(3)[:, 0:128], ckv[:, t, 0:128], ident), reads=["ckv%d" % t, "ident"], writes=["ps3"])
            yield
            P.op("act", lambda e: e.copy(out=ckvT[:, t * 128:(t + 1) * 128], in_=psh(3)[:, 0:128]), reads=["ps3"], writes=["ckvT%d" % t])
            yield
            if tl == 3:
                for ci in range(9):
                    pb = 4 + ci % 3
                    if ci < 8:
                        c0, M = ci * 128, 128
                    else:
                        c0, M = 1664, 64

                    def mmf(e, c0=c0, M=M, pb=pb):
                        ins = None
                        for kc in range(8):
                            ins = e.matmul(psf(pb)[0:M, :], w1[:, kc, c0:c0 + M], hTg[:, kc, :], start=(kc == 0), stop=(kc == 7))
                        return ins
                    P.op("pe", mmf, reads=[kh, "w1"], writes=["ps%d" % pb])
                    if ci < 4:
                        dst, key = qaT[:, ci, g * 512:(g + 1) * 512], "qaT"
                    elif ci < 8:
                        dst, key = kaT[:, ci - 4, g * 512:(g + 1) * 512], "kaT"
                    else:
                        dst, key = kiT[0:64, g * 512:(g + 1) * 512], "kiT"
                    if ci % 2 == 0:
                        P.op("act", lambda e, dst=dst, pb=pb, M=M: e.copy(out=dst, in_=psf(pb)[0:M, :]), reads=["ps%d" % pb], writes=[key + str(g)])
                    else:
                        P.op("dve", lambda e, dst=dst, pb=pb, M=M: e.tensor_copy(out=dst, in_=psf(pb)[0:M, :]), reads=["ps%d" % pb], writes=[key + str(g)])
                    yield

        for t in range(NT + 2):
            gx = tile1(t, 0) if t < NT else None
            gx2 = tile1(t - 1, 1) if 1 <= t <= NT else None
            gy = tile1(t - 2, 2) if t >= 2 else None
            interleave(gx, 3, gx2, 3, gy, 15 if (t - 2) % 4 == 3 else 6)
        if debug:
            P.barrier()
            P.dma("sp", dbg["kaT"], kaT.rearrange("p c t -> p (c t)"), writes=["dbg1"])
            P.dma("sp", dbg["ckv"], ckv.rearrange("p c t -> p (c t)"), writes=["dbg2"])
            P.dma("sp", dbg["ckvT"], ckvT, writes=["dbg3"])
        P.barrier()

    if 2 in phases:
        ar.off = p2a_off
        phase2a(P, ar, psf, psh, qaT, kaT, V_d, pA_d, cst_d)
        P.barrier()
        ar.off = p2b_off
        phase2b(P, ar, psf, psh, locals())
        P.barrier()
    else:
        xb = ar.alloc([128, D], F32)
        x_v = x_d.rearrange("(n p) d -> n p d", p=128)
        x1_v = x1_d.rearrange("(n p) d -> n p d", p=128)
        for t in range(NT):
            P.dma("sp", xb, x_v[t], writes=["xb"])
            P.dma("sp", x1_v[t], xb, reads=["xb"], writes=["x1_d"])
        P.barrier()

    ar.off = p3_off
    if 3 in phases:
        GT = 256
        NTG = GT // 128
        wgu = ar.alloc([128, 8, 2 * DFF], BF16)
        wdn = ar.alloc([128, NJ, D], BF16)
        p3w_off = ar.off
        woutc = ar.alloc([128, 8, D], BF16)
        mixt = [ar.alloc([128, D], BF16) for _ in range(2)]
        mixT = [ar.alloc([128, 8, 128], BF16) for _ in range(2)]
        xin = [ar.alloc([128, D], F32) for _ in range(2)]
        tmpc = ar.alloc([128, D], F32)
        x1o = [ar.alloc([128, D], F32) for _ in range(2)]
        ar.off = p3w_off
        x1t = [ar.alloc([128, D], F32) for _ in range(4)]
        h2 = ar.alloc([128, D], BF16)
        tmpA = ar.alloc([128, D], F32)
        h2T = [ar.alloc([128, 8, GT], BF16) for _ in range(2)]
        actT = ar.alloc([128, NJ, GT], BF16)
        sg = [ar.alloc([128, GT], F32) for _ in range(2)]
        x2 = ar.alloc([128, D], F32)
        outt = ar.alloc([128, D], F32)
        junk3 = ar.alloc([128, D], BF16)
        st3 = ar.alloc([128, 8], F32)
        nh3 = ar.alloc([128, 1], F32)
        P.dma("sp", woutc, wout_s, writes=["woutc"])
        wloads = []
        for pc in range(DFF // WP):
            for gu in range(2):
                c0 = gu * DFF + pc * WP
                wloads.append((wgu[:, :, c0:c0 + WP], wgu_s[:, :, c0:c0 + WP], "wgu%d_%d" % (gu, pc)))
        for jp in range(0, NJ, 2):
            wloads.append((wdn[:, jp:jp + 2, :], wdn_s[:, jp:jp + 2, :], "wdn%d" % (jp // 2)))
        assert 2 in phases
        x1_v = x1_d.rearrange("(n p) d -> n p d", p=128)
        out_v = out_d.rearrange("(n p) d -> n p d", p=128)
        if 2 in phases:
            x_v3 = x_d.rearrange("(n p) d -> n p d", p=128)
            mix_v3 = mix_d.rearrange("(n p) d -> n p d", p=128)
            def c2x(t):
                s = t % 2
                if wloads:
                    o_, i_, k_ = wloads.pop(0)
                    P.dma("sp", o_, i_, writes=[k_])
                P.dma("sp", mixt[s], mix_v3[t], writes=["mixt%d" % s])
                P.dma("sp", xin[s], x_v3[t], writes=["xin%d" % s])
                yield

                def trm(e, s=s):
                    ins = None
                    for c in range(8):
                        ins = e.transpose(psh(t % 2)[:, c * 128:(c + 1) * 128], mixt[s][:, c * 128:(c + 1) * 128], ident)
                    return ins
                P.op("pe", trm, reads=["mixt%d" % s], writes=["ps%d" % (t % 2)])
                yield
                P.op("act", lambda e: e.copy(out=mixT[s], in_=psh(t % 2).rearrange("p (c t) -> p c t", t=128)), reads=["ps%d" % (t % 2)], writes=["mixT%d" % s])
                yield

            def c2y(t):
                s = t % 2
                for half in range(2):
                    def my(e, half=half):
                        ins = None
                        for c in range(8):
                            ins = e.matmul(psf(2 + half), mixT[s][:, c, :], woutc[:, c, half * 512:(half + 1) * 512], start=(c == 0), stop=(c == 7))
                        return ins
                    P.op("pe", my, reads=["mixT%d" % s, "woutc"], writes=["ps%d" % (2 + half)])
                    P.op("dve", lambda e, half=half: e.tensor_tensor(out=tmpc[:, half * 512:(half + 1) * 512], in0=psf(2 + half),
                                                                     in1=GA1[:, half * 512:(half + 1) * 512], op=ALU.mult),
                         reads=["ps%d" % (2 + half)], writes=["tmpc"])
                    yield
                P.op("pool", lambda e: e.tensor_tensor(out=x1o[s], in0=tmpc, in1=xin[s], op=ALU.add), reads=["tmpc", "xin%d" % s], writes=["x1o%d" % s])
                P.dma("pool", x1_v[t], x1o[s], reads=["x1o%d" % s], writes=["x1_d"])
                yield
            for t in range(NT + 1):
                interleave(c2x(t) if t < NT else None, 3, c2y(t - 1) if t >= 1 else None, 3)
            for o_, i_, k_ in wloads:
                P.dma("sp", o_, i_, writes=[k_])
            wloads = []
            P.barrier()
        for o_, i_, k_ in wloads:
            P.dma("sp", o_, i_, writes=[k_])
        P.op("pool", lambda e: e.memset(nh3, -0.5), writes=["nhalf"])

        def wkeys(gu, j):
            c0, c1 = j * 128, (j + 1) * 128 - 1
            return ["wgu%d_%d" % (gu, p) for p in range(c0 // WP, c1 // WP + 1)]

        def stageN(g):
            hTg = h2T[g % 2]
            kh = "h2T%d" % (g % 2)
            for tl in range(NTG):
                t = NTG * g + tl
                s = t % 4
                P.dma("sp", x1t[s], x1_v[t], reads=["x1_d"], writes=["x1t%d" % s])
                P.op("act", lambda e, s=s: e.activation(out=junk3, in_=x1t[s], func=AF.Square, accum_out=st3[:, 0:1]),
                     reads=["x1t%d" % s], writes=["junk3", "s30"])
                rstd_ops(P, st3[:, 0:1], st3[:, 1:2], st3[:, 2:3], nh3, 1.0 / D, ("s30", "s31", "s32"))
                yield
                P.op("dve", lambda e, s=s: e.scalar_tensor_tensor(out=tmpA, in0=x1t[s], scalar=st3[:, 2:3], op0=ALU.mult,
                                                                    in1=GM2, op1=ALU.mult),
                     reads=["x1t%d" % s, "s32", "modB"], writes=["tmpA"])
                yield
                P.op("pool", lambda e: e.tensor_tensor(out=h2, in0=tmpA, in1=SH2, op=ALU.add),
                     reads=["tmpA", "modB"], writes=["h2"])

                def tr(e):
                    ins = None
                    for c in range(8):
                        ins = e.transpose(psh(0)[:, c * 128:(c + 1) * 128], h2[:, c * 128:(c + 1) * 128], ident)
                    return ins
                P.op("pe", tr, reads=["h2", "ident"], writes=["ps0"])
                yield
                P.op("act", lambda e, tl=tl, hTg=hTg: e.copy(out=hTg[:, :, tl * 128:(tl + 1) * 128],
                                                              in_=psh(0).rearrange("p (c t) -> p c t", t=128)),
                     reads=["ps0"], writes=[kh])
                yield

        def stageM(g):
            hTg = h2T[g % 2]
            kh = "h2T%d" % (g % 2)
            for j in range(NJ):
                pg = 1 + (j % 2) * 2
                pu = pg + 1

                def mg(e, j=j, pg=pg, pu=pu, hTg=hTg):
                    ins = None
                    for kc in range(8):
                        ins = e.matmul(psf(pg)[:, 0:GT], wgu[:, kc, j * 128:(j + 1) * 128], hTg[:, kc, :],
                                       start=(kc == 0), stop=(kc == 7))
                    for kc in range(8):
                        ins = e.matmul(psf(pu)[:, 0:GT], wgu[:, kc, DFF + j * 128:DFF + (j + 1) * 128], hTg[:, kc, :],
                                       start=(kc == 0), stop=(kc == 7))
                    return ins
                P.op("pe", mg, reads=[kh] + wkeys(0, j) + wkeys(1, j), writes=["ps%d" % pg, "ps%d" % pu])
                sj = sg[j % 2]
                P.op("act", lambda e, pg=pg, sj=sj: e.activation(out=sj, in_=psf(pg)[:, 0:GT], func=AF.Silu),
                     reads=["ps%d" % pg], writes=["sg%d" % (j % 2)])
                P.op("dve", lambda e, j=j, pu=pu, sj=sj: e.tensor_tensor(out=actT[:, j, :], in0=psf(pu)[:, 0:GT], in1=sj, op=ALU.mult),
                     reads=["ps%d" % pu, "sg%d" % (j % 2)], writes=["actT"])
                yield
            for tl in range(NTG):
                t = NTG * g + tl
                s = t % 4
                for hf in range(2):
                    pb = 5 + hf

                    def md(e, tl=tl, hf=hf, pb=pb):
                        ins = None
                        for j in range(NJ):
                            ins = e.matmul(psf(pb), actT[:, j, tl * 128:(tl + 1) * 128], wdn[:, j, hf * 512:(hf + 1) * 512],
                                           start=(j == 0), stop=(j == NJ - 1))
                        return ins
                    P.op("pe", md, reads=["actT"] + ["wdn%d" % k for k in range(NJ // 2)], writes=["ps%d" % pb])
                    P.op("dve", lambda e, hf=hf, pb=pb: e.tensor_tensor(out=x2[:, hf * 512:(hf + 1) * 512], in0=psf(pb),
                                                                         in1=GA2[:, hf * 512:(hf + 1) * 512], op=ALU.mult),
                         reads=["ps%d" % pb, "modB"], writes=["x2"])
                    yield
                P.op("pool", lambda e, s=s: e.tensor_tensor(out=x2, in0=x2, in1=x1t[s], op=ALU.add),
                     reads=["x2", "x1t%d" % s], writes=["x2"])
                P.op("act", lambda e: e.activation(out=junk3, in_=x2, func=AF.Square, accum_out=st3[:, 3:4]),
                     reads=["x2"], writes=["junk3", "s33"])
                rstd_ops(P, st3[:, 3:4], st3[:, 4:5], st3[:, 5:6], nh3, 1.0 / D, ("s33", "s34", "s35"))
                yield
                P.op("dve", lambda e: e.scalar_tensor_tensor(out=outt, in0=x2, scalar=st3[:, 5:6], op0=ALU.mult, in1=gfB, op1=ALU.mult),
                     reads=["x2", "s35", "gfB"], writes=["outt"])
                P.dma("sp", out_v[t], outt, reads=["outt"], writes=["out_d"])
                yield

        NG = T // GT
        for g in range(NG + 1):
            ga = stageN(g) if g < NG else None
            gb = stageM(g - 1) if g >= 1 else None
            interleave(ga, 4 * NTG, gb, NJ + 4 * NTG)
    P.barrier()
    P.emit()
    es.close()
    return nc, consts, list(dbg.keys())


_CACHE = {}


def kernel(x, c, w_ada, b_ada, g_attn, w_in, kv_norm_g, w_uk, w_uv, w_out, g_ffn, w_gu, w_down, g_final, _debug=False, _phases=(0, 1, 2, 3)):
    key = (bool(_debug), tuple(_phases))
    if key not in _CACHE:
        _CACHE[key] = build(debug=_debug, phases=_phases)
    nc, consts, dbgk = _CACHE[key]
    f = lambda a: np.ascontiguousarray(np.asarray(a, dtype=np.float32))
    x = f(x); c = f(c)
    shared = {
        "w_ada": f(w_ada)[0], "b_ada": f(b_ada)[0][None, :], "g_attn": f(g_attn)[0][None, :], "w_in": f(w_in)[0],
        "kv_norm_g": f(kv_norm_g)[0][None, :], "w_uk": f(w_uk)[0], "w_uv": f(w_uv)[0], "w_out": f(w_out)[0],
        "g_ffn": f(g_ffn)[0][None, :], "w_gu": f(w_gu)[0], "w_down": f(w_down)[0], "g_final": f(g_final)[None, :],
    }
    for k, v in consts.items():
        shared["k_" + k] = np.ascontiguousarray(v)
    in_maps = []
    for b in range(8):
        m = dict(shared)
        m["x"] = x[b]
        m["c"] = np.ascontiguousarray(c[b].reshape(8, 128).T)
        in_maps.append(m)
    res = run_bass_kernel_spmd(nc, in_maps, core_ids=list(range(8)))
    out = np.stack([np.asarray(r["out"], dtype=np.float32) for r in res.results], axis=0)
    if _debug:
        return out, res.results
    return out
```

```python
import numpy as np
import concourse.bass as bass
import concourse.mybir as mybir
from concourse.bass_utils import run_bass_kernel_spmd

F32 = mybir.dt.float32
BF16 = mybir.dt.bfloat16
U8 = mybir.dt.uint8
AF = mybir.ActivationFunctionType
ALU = mybir.AluOpType
AX = mybir.AxisListType

D = 1024
T = 4096
NT = T // 128
DFF = 2816
NJ = DFF // 128
EPS = 1e-6
NEG = -30000.0
IDX_SCALE = 512.0 ** -0.5
TOPK = 256
NBIS = 13

COMPUTE = ("pe", "act", "dve", "pool")
CH = 16384
NDMA = 8


class Prog:
    def __init__(self, nc, es):
        self.nc = nc
        self.es = es
        self.engs = {"pe": nc.tensor, "act": nc.scalar, "dve": nc.vector, "pool": nc.gpsimd, "sp": nc.sync}
        self.ops = {e: [] for e in self.engs}
        self.cnt = {}
        self.sems = {}
        self.last_w = {}
        self.readers = {}
        self.seen = {e: {} for e in self.engs}
        self.dma_n = {e: 0 for e in self.engs}

    def _sem(self, name):
        if name not in self.sems:
            self.sems[name] = self.es.enter_context(self.nc.semaphore("s_" + name))
        return self.sems[name]

    def _tok_wait(self, tok):
        src, idx = tok
        if src[0] == "dma":
            return (self._sem("d_%s_%d" % (src[1], src[2])), 16 * (idx + 1))
        return (self._sem("%s_%d" % (src[0], idx // CH)), idx % CH + 1)

    def _need(self, eng, tok, waits):
        if tok is None:
            return
        src, idx = tok
        if self.seen[eng].get(src, -1) >= idx:
            return
        self.seen[eng][src] = idx
        waits.append(self._tok_wait(tok))

    def _deps(self, eng, reads, writes, is_dma=False):
        waits = []
        for k in reads:
            tok = self.last_w.get(k)
            if tok is not None and not (tok[0] == ("pe",) and eng == "pe"):
                self._need(eng, tok, waits)
        for k in writes:
            tok = self.last_w.get(k)
            if tok is not None and (is_dma or tok[0] != (eng,)):
                self._need(eng, tok, waits)
            for r in self.readers.get(k, ()):
                if is_dma or r[0] != (eng,):
                    self._need(eng, r, waits)
        return waits

    def op(self, eng, fn, reads=(), writes=()):
        waits = self._deps(eng, reads, writes)
        idx = self.cnt.get(eng, 0)
        self.cnt[eng] = idx + 1
        tok = ((eng,), idx)
        inc = (self._sem("%s_%d" % (eng, idx // CH)), 1)
        self.ops[eng].append((waits, fn, inc))
        for k in reads:
            self.readers.setdefault(k, []).append(tok)
        for k in writes:
            self.last_w[k] = tok
            self.readers[k] = []
        return tok

    def dma(self, q, out, in_, reads=(), writes=()):
        waits = self._deps(q, reads, writes, is_dma=True)
        n = self.dma_n[q]
        self.dma_n[q] = n + 1
        slot = n % NDMA
        idx = n // NDMA
        src = ("dma", q, slot)
        if idx > 0:
            self._need(q, (src, idx - 1), waits)
        tok = (src, idx)
        inc = (self._sem("d_%s_%d" % (q, slot)), 16)
        self.ops[q].append((waits, lambda e: e.dma_start(out=out, in_=in_), inc))
        for k in reads:
            self.readers.setdefault(k, []).append(tok)
        for k in writes:
            self.last_w[k] = tok
            self.readers[k] = []
        return tok

    def barrier(self):
        toks = []
        for e in COMPUTE:
            if self.cnt.get(e, 0) > 0:
                toks.append(((e,), self.cnt[e] - 1))
        for q in self.engs:
            n = self.dma_n[q]
            for slot in range(min(n, NDMA)):
                cntslot = (n - slot + NDMA - 1) // NDMA
                if cntslot > 0:
                    toks.append((("dma", q, slot), cntslot - 1))
        for e in self.engs:
            waits = []
            for tok in toks:
                self._need(e, tok, waits)
            if waits:
                self.ops[e].append((waits, None, None))
        self.last_w = {}
        self.readers = {}

    def emit(self):
        with self.nc.Block() as block:
            def mk(ename):
                def body(eng):
                    for waits, fn, inc in self.ops[ename]:
                        for s, v in waits:
                            eng.wait_ge(s, v)
                        if fn is not None:
                            ins = fn(eng)
                            ins.then_inc(inc[0], inc[1])
                return body
            block.tensor(mk("pe"))
            block.scalar(mk("act"))
            block.vector(mk("dve"))
            block.gpsimd(mk("pool"))
            block.sync(mk("sp"))


class Arena:
    def __init__(self, t, size):
        self.t = t
        self.size = size
        self.off = 0

    def alloc_at(self, off, shape, dtype):
        save = self.off
        self.off = off
        v = self.alloc(shape, dtype)
        end = self.off
        self.off = save
        return v, end

    def alloc(self, shape, dtype):
        esz = mybir.dt.size(dtype)
        n = 1
        for s in shape[1:]:
            n *= s
        nbytes = (n * esz + 31) // 32 * 32
        assert self.off + nbytes <= self.size, ("arena overflow", self.off, nbytes, self.size)
        v = self.t[0:128, self.off:self.off + nbytes]
        if dtype != U8:
            v = v.bitcast(dtype)
        v = v[:, 0:n]
        self.off += nbytes
        if len(shape) == 3:
            v = v.rearrange("p (a b) -> p a b", b=shape[2])
        elif len(shape) == 4:
            v = v.rearrange("p (a b c) -> p a b c", b=shape[2], c=shape[3])
        if shape[0] != 128:
            v = v[0:shape[0]]
        return v


def alibi_slopes():
    s = 2.0 ** (-8.0 * (np.arange(16, dtype=np.float32) + 1.0) / 16)
    return s[0::2].astype(np.float32), s[1::2].astype(np.float32)


def make_consts():
    sa, sb = alibi_slopes()
    c = {}
    c["ident"] = np.eye(128, dtype=np.float32)
    c["identrep"] = np.tile(np.eye(128, dtype=np.float32), (1, 4))
    ik = np.arange(128)[:, None]
    iq = np.arange(128)[None, :]
    bA = np.zeros((128, 3, 2, 8, 128), np.float32)
    for ci, dil in enumerate((1, 4, 16)):
        for h in range(8):
            dprev = iq - ik + 128
            bA[:, ci, 0, h, :] = np.where(dprev <= 128, -sa[h] * dil * dprev, NEG)
            dcur = iq - ik
            bA[:, ci, 1, h, :] = np.where(dcur >= 0, -sa[h] * dil * dcur, NEG)
    c["biasA"] = bA.reshape(128, -1)
    al = np.zeros((3, 32, 128), np.float32)
    for d in range(32):
        al[0, d, :] = d
        al[1, d, :] = 1.0
        al[2, d, :] = np.arange(128)
    c["alL"] = al.reshape(3, -1)
    ar = np.zeros((3, 8, 128), np.float32)
    for h in range(8):
        ar[0, h, :] = -1024.0 * sb[h]
        ar[1, h, :] = -8.0 * sb[h] * np.arange(128)
        ar[2, h, :] = 8.0 * sb[h]
    c["alR"] = ar.reshape(3, -1)
    cm = np.where(np.arange(128)[None, :] <= np.arange(128)[:, None], 0.0, -1e30).astype(np.float32)
    c["causal"] = cm
    c["pow2"] = np.tile((2.0 ** -np.arange(0, NBIS + 2, dtype=np.float32))[None, :], (128, 1)).astype(np.float32)
    return c


def pipeline(gens, depth):
    active = []
    nxt = 0
    while active or nxt < len(gens):
        while len(active) < depth and nxt < len(gens):
            active.append(gens[nxt])
            nxt += 1
        for g in list(active):
            try:
                next(g)
            except StopIteration:
                active.remove(g)


def interleave(*gw):
    gens = [[g, max(1, n), 0] for g, n in zip(gw[0::2], gw[1::2]) if g is not None]
    while gens:
        best = min(gens, key=lambda x: x[2] / x[1])
        try:
            next(best[0])
            best[2] += 1
        except StopIteration:
            gens.remove(best)


def phase2a(P, ar, psf, psh, qaT, kaT, V_d, pA_d, cst_d):
    biasA = ar.alloc([128, 6, 8, 128], F32)
    Ssb = [[[ar.alloc([128, 4, 128], F32) for _ in range(2)] for _ in range(2)] for _ in range(2)]
    PT = [[[ar.alloc([128, 512], BF16) for _ in range(2)] for _ in range(2)] for _ in range(2)]
    Vb = [ar.alloc([128, 8, 65], BF16) for _ in range(3)]
    ob = [ar.alloc([128, 520], F32) for _ in range(2)]
    P.dma("sp", biasA.rearrange("p a h q -> p (a h q)"), cst_d["biasA"], writes=["biasA"])
    blocks = []
    for ci, dil in enumerate((1, 4, 16)):
        nb = T // dil // 128
        for r in range(dil):
            for n in range(nb):
                blocks.append((ci, dil, r, n))
    nblk = len(blocks)

    def tsl(dil, r, n):
        base = r + dil * 128 * n
        return slice(base, base + dil * 127 + 1, dil)

    for idx in range(nblk + 1):
        if idx < nblk:
            ci, dil, r, n = blocks[idx]
            bp = idx % 2
            vs = idx % 3
            P.dma("sp", Vb[vs].rearrange("p h d -> p (h d)"), V_d[tsl(dil, r, n), :], writes=["Vb%d" % vs])
            whichs = (1,) if n == 0 else (0, 1)
            for which in whichs:
                kn = n - 1 if which == 0 else n
                for par in range(2):
                    bank = which * 2 + par

                    def qk(e, dil=dil, r=r, n=n, kn=kn, par=par, bank=bank):
                        ins = None
                        p0 = par * 64
                        for hh in range(4):
                            ins = e.matmul(psf(bank)[:, hh * 128:(hh + 1) * 128],
                                           kaT[p0:p0 + 64, hh, tsl(dil, r, kn)], qaT[p0:p0 + 64, hh, tsl(dil, r, n)],
                                           start=True, stop=True)
                        return ins
                    P.op("pe", qk, writes=["ps%d" % bank])
                    sb = Ssb[bp][which][par]
                    P.op("dve", lambda e, sb=sb, bank=bank, ci=ci, which=which, par=par: e.scalar_tensor_tensor(
                        out=sb, in0=psf(bank).rearrange("p (a b) -> p a b", b=128), scalar=0.125, op0=ALU.mult,
                        in1=biasA[:, ci * 2 + which, par::2, :], op1=ALU.add),
                        reads=["ps%d" % bank, "biasA"], writes=["Ssb%d%d%d" % (bp, which, par)])
                    pt = PT[bp][which][par]
                    P.op("act", lambda e, sb=sb, pt=pt: e.activation(out=pt, in_=sb.rearrange("p a b -> p (a b)"), func=AF.Exp),
                         reads=["Ssb%d%d%d" % (bp, which, par)], writes=["PT%d%d%d" % (bp, which, par)])
        if idx >= 1:
            j = idx - 1
            ci, dil, r, n = blocks[j]
            bp = j % 2
            whichs = (1,) if n == 0 else (0, 1)
            oa, obk = (4, 5) if bp == 0 else (6, 7)

            def pv(e, bp=bp, j=j, whichs=whichs, oa=oa, obk=obk):
                ins = None
                for h in range(8):
                    par, hh = h % 2, h // 2
                    bank = oa if h < 4 else obk
                    col = (h % 4) * 65
                    for wi_, which in enumerate(whichs):
                        vsl = (j - 1) % 3 if which == 0 else j % 3
                        ins = e.matmul(psf(bank)[:, col:col + 65], PT[bp][which][par][:, hh * 128:(hh + 1) * 128],
                                       Vb[vsl][:, h, :], start=(wi_ == 0), stop=(wi_ == len(whichs) - 1))
                return ins
            rd = ["PT%d%d%d" % (bp, w, p) for w in whichs for p in range(2)] + ["Vb%d" % (j % 3)]
            if n > 0:
                rd.append("Vb%d" % ((j - 1) % 3))
            P.op("pe", pv, reads=rd, writes=["ps%d" % oa, "ps%d" % obk])
            o = ob[bp]
            P.op("act", lambda e, o=o, oa=oa: e.copy(out=o[:, 0:260], in_=psf(oa)[:, 0:260]), reads=["ps%d" % oa], writes=["ob%da" % bp])
            P.op("dve", lambda e, o=o, obk=obk: e.tensor_copy(out=o[:, 260:520], in_=psf(obk)[:, 0:260]), reads=["ps%d" % obk], writes=["ob%db" % bp])
            P.dma("sp", pA_d[ci, tsl(dil, r, n), :], o, reads=["ob%da" % bp, "ob%db" % bp], writes=["pA_d"])


def rstd_ops(P, ss, ms, rstd, nhalf, scale, keys):
    k_ss, k_ms, k_r = keys
    P.op("dve", lambda e: e.tensor_scalar(out=ms, in0=ss, scalar1=scale, scalar2=EPS, op0=ALU.mult, op1=ALU.add), reads=[k_ss], writes=[k_ms])
    P.op("pool", lambda e: e.tensor_tensor(out=rstd, in0=ms, in1=nhalf, op=ALU.pow), reads=[k_ms, "nhalf"], writes=[k_r])


def phase2b(P, ar, psf, psh, L):
    x_d, mix_d, pA_d, cst_d = L["x_d"], L["mix_d"], L["pA_d"], L["cst_d"]
    win_d, wuk_d, wuv_d = L["win_d"], L["wuk_d"], L["wuv_d"]
    kiT, ckv, ckvT, ident = L["kiT"], L["ckv"], L["ckvT"], L["ident"]
    SH1, GM1 = L["SH1"], L["GM1"]
    w2 = ar.alloc([128, 8, 1032], BF16)
    wuk = ar.alloc([128, 4, 128], BF16)
    wuv = ar.alloc([128, 8, 64], BF16)
    identrep = ar.alloc([128, 512], BF16)
    alL = ar.alloc([128, 32, 128], BF16)
    alR = ar.alloc([128, 1024], BF16)
    causal = ar.alloc([128, 128], F32)
    pow2 = ar.alloc([128, NBIS + 2], F32)
    nhalf = ar.alloc([128, 1], F32)
    xq = ar.alloc([128, D], F32)
    junk = ar.alloc([128, D], BF16)
    hb = ar.alloc([128, D], BF16)
    tmpf = ar.alloc([128, D], F32)
    hTq = ar.alloc([128, 8, 128], BF16)
    qbT = ar.alloc([128, 4, 128], BF16)
    qis = ar.alloc([128, 8, 64], BF16)
    qiT = ar.alloc([128, 8, 128], BF16)
    wis = ar.alloc([128, 8], F32)
    cmax = [ar.alloc([128, 8], F32) for _ in range(2)]
    dg = ar.alloc([128, 8, 128], BF16)
    rl = [ar.alloc([128, 512], BF16) for _ in range(4)]
    st = ar.alloc([128, 8], F32)
    stb = ar.alloc([128, 8], F32)
    wk = ar.alloc([128, NBIS + 2], F32)
    score = [ar.alloc([128, T], F32) for _ in range(2)]
    mb = [ar.alloc([128, T], BF16) for _ in range(2)]
    qlT = [ar.alloc([128, 8, 128], BF16) for _ in range(3)]
    thr = [ar.alloc([128, 1], F32) for _ in range(2)]
    PT = [[ar.alloc([128, 512], BF16) for _ in range(2)] for _ in range(2)]
    rec = ar.alloc([128, 8], F32)
    recA = ar.alloc([128, 8], F32)
    olat = ar.alloc([128, 8, 128], BF16)
    olatT = ar.alloc([128, 8, 128], BF16)
    mixed = [ar.alloc([128, D], BF16) for _ in range(2)]
    pAt = ar.alloc([128, 3, 520], F32)
    tot = ar.alloc([128, 8, 65], F32)

    win_v = win_d.rearrange("(c p) n -> p c n", p=128)
    P.dma("pool", w2[:, :, 0:512], win_v[:, :, 1536:2048], writes=["w2"])
    P.dma("pool", w2[:, :, 512:1024], win_v[:, :, 2176:2688], writes=["w2"])
    P.dma("pool", w2[:, :, 1024:1032], win_v[:, :, 2752:2760], writes=["w2"])
    P.dma("pool", wuk, wuk_d.rearrange("(c two) d r -> (two d) c r", two=2), writes=["wuk"])
    P.dma("pool", wuv, wuv_d.rearrange("h r d -> r h d"), writes=["wuv"])
    P.dma("pool", identrep, cst_d["identrep"], writes=["identrep"])
    P.op("pool", lambda e: e.memset(alL, 0.0), writes=["alL"])
    P.op("pool", lambda e: e.memset(alR, 0.0), writes=["alR"])
    P.dma("pool", alL[0:3].rearrange("p a b -> p (a b)"), cst_d["alL"], writes=["alL"])
    P.dma("pool", alR[0:3], cst_d["alR"], writes=["alR"])
    P.dma("sp", causal, cst_d["causal"], writes=["causal"])
    P.dma("sp", pow2, cst_d["pow2"], writes=["pow2"])
    P.op("pool", lambda e: e.memset(nhalf, -0.5), writes=["nhalf"])
    P.op("pool", lambda e: e.memset(qiT[64:128], 0.0), writes=["qiT_z"])
    x_v = x_d.rearrange("(n p) d -> n p d", p=128)
    mix_v = mix_d.rearrange("(n p) d -> n p d", p=128)
    assert len(L["precast"]) <= NT + 2

    def stageA1(i):
        sp_ = i % 2
        q3 = i % 3
        sc = score[sp_]
        ksc = "score%d" % sp_
        N = 128 * (i + 1)
        P.dma("sp", xq, x_v[i], writes=["xq"])
        P.op("act", lambda e: e.activation(out=junk, in_=xq, func=AF.Square, accum_out=st[:, 0:1]), reads=["xq"], writes=["junk", "st0"])
        rstd_ops(P, st[:, 0:1], st[:, 1:2], st[:, 2:3], nhalf, 1.0 / D, ("st0", "st1", "st2"))
        P.op("dve", lambda e: e.scalar_tensor_tensor(out=tmpf, in0=xq, scalar=st[:, 2:3], op0=ALU.mult, in1=GM1, op1=ALU.mult),
             reads=["xq", "st2"], writes=["tmpf"])
        yield
        P.op("pool", lambda e: e.tensor_tensor(out=hb, in0=tmpf, in1=SH1, op=ALU.add), reads=["tmpf"], writes=["hb"])

        def tr(e):
            ins = None
            for c in range(8):
                ins = e.transpose(psh(0)[:, c * 128:(c + 1) * 128], hb[:, c * 128:(c + 1) * 128], ident)
            return ins
        P.op("pe", tr, reads=["hb"], writes=["ps0"])
        P.op("act", lambda e: e.copy(out=hTq, in_=psh(0).rearrange("p (c t) -> p c t", t=128)), reads=["ps0"], writes=["hTq"])
        yield

        def mqb(e):
            ins = None
            for cc in range(4):
                for kc in range(8):
                    ins = e.matmul(psf(1)[:, cc * 128:(cc + 1) * 128], w2[:, kc, cc * 128:(cc + 1) * 128], hTq[:, kc, :],
                                   start=(kc == 0), stop=(kc == 7))
            return ins
        P.op("pe", mqb, reads=["hTq", "w2"], writes=["ps1"])
        P.op("act", lambda e: e.copy(out=qbT, in_=psf(1).rearrange("p (c t) -> p c t", t=128)), reads=["ps1"], writes=["qbT"])

        def mqi(e):
            ins = None
            for kc in range(8):
                ins = e.matmul(psf(2), hTq[:, kc, :], w2[:, kc, 512:1024], start=(kc == 0), stop=(kc == 7))
            return ins
        P.op("pe", mqi, reads=["hTq", "w2"], writes=["ps2"])

        def mwi(e):
            ins = None
            for kc in range(8):
                ins = e.matmul(psf(0)[:, 0:8], hTq[:, kc, :], w2[:, kc, 1024:1032], start=(kc == 0), stop=(kc == 7))
            return ins
        P.op("pe", mwi, reads=["hTq", "w2"], writes=["ps0"])
        yield
        P.op("act", lambda e: e.activation(out=wis, in_=psf(0)[:, 0:8], func=AF.Copy, scale=IDX_SCALE), reads=["ps0"], writes=["wis"])
        P.op("dve", lambda e: e.tensor_tensor(out=dg, in0=ident.unsqueeze(1).broadcast_to([128, 8, 128]),
                                              in1=wis.unsqueeze(2).broadcast_to([128, 8, 128]), op=ALU.mult),
             reads=["wis"], writes=["dg"])
        P.op("act", lambda e: e.copy(out=qis, in_=psf(2).rearrange("p (h d) -> p h d", d=64)), reads=["ps2"], writes=["qis"])

        def tq(e):
            ins = None
            for s_ in range(8):
                ins = e.transpose(psh(2)[0:64, s_ * 128:(s_ + 1) * 128], qis[:, s_, :], ident)
            return ins
        P.op("pe", tq, reads=["qis"], writes=["ps2"])
        P.op("act", lambda e: e.copy(out=qiT[0:64], in_=psh(2)[0:64, :].rearrange("p (c t) -> p c t", t=128)), reads=["ps2"], writes=["qiT"])
        yield

        def mql(e):
            ins = None
            for h in range(8):
                p0 = (h % 2) * 64
                ins = e.matmul(psf(1 + h % 2)[:, (h // 2) * 128:(h // 2 + 1) * 128], wuk[p0:p0 + 64, h // 2, :], qbT[p0:p0 + 64, h // 2, :],
                               start=True, stop=True)
            return ins
        P.op("pe", mql, reads=["qbT", "wuk"], writes=["ps1", "ps2"])
        P.op("act", lambda e: e.copy(out=qlT[q3][:, 0::2, :], in_=psf(1).rearrange("p (c t) -> p c t", t=128)), reads=["ps1"], writes=["qlTa%d" % q3])
        P.op("dve", lambda e: e.tensor_copy(out=qlT[q3][:, 1::2, :], in_=psf(2).rearrange("p (c t) -> p c t", t=128)), reads=["ps2"], writes=["qlTb%d" % q3])
        yield
        nch = (N + 511) // 512
        for c in range(nch):
            W = min(512, N - 512 * c)
            ksl = slice(512 * c, 512 * c + W)

            def emit_L(h, ksl=ksl, W=W):
                b = 1 + h % 2
                P.op("pe", lambda e, h=h, b=b: e.matmul(psf(b)[:, 0:W], qiT[:, h, :], kiT[:, ksl], start=True, stop=True),
                     reads=["qiT"], writes=["ps%d" % b])
                rs = rl[h % 4]
                P.op("act", lambda e, b=b, rs=rs: e.activation(out=rs[:, 0:W], in_=psf(b)[:, 0:W], func=AF.Relu),
                     reads=["ps%d" % b], writes=["rl%d" % (h % 4)])

            def emit_D(h, W=W):
                rs = rl[h % 4]
                P.op("pe", lambda e, h=h, rs=rs: e.matmul(psf(0)[:, 0:W], dg[:, h, :], rs[:, 0:W], start=(h == 0), stop=(h == 7)),
                     reads=["dg", "rl%d" % (h % 4)], writes=["ps0"])
            emit_L(0)
            emit_L(1)
            for h in range(8):
                if h + 2 < 8:
                    emit_L(h + 2)
                emit_D(h)
                if h % 2 == 1:
                    yield
            P.op("dve", lambda e, ksl=ksl, W=W, c=c: e.tensor_scalar(out=sc[:, ksl], in0=psf(0)[:, 0:W], scalar1=1.0, scalar2=None, op0=ALU.mult, op1=ALU.max,
                                                                       accum_out=cmax[sp_][:, c:c + 1]),
                 reads=["ps0"], writes=[ksc, "cmax%d" % sp_])
            yield

    def stageA2(i):
        sp_ = i % 2
        par = i % 2
        sc = score[sp_]
        ksc = "score%d" % sp_
        N = 128 * (i + 1)
        nch = (N + 511) // 512
        if i >= 2:
            P.op("dve", lambda e: e.tensor_reduce(out=stb[:, 0:1], in_=cmax[sp_][:, 0:nch], axis=AX.X, op=ALU.max), reads=["cmax%d" % sp_], writes=["rmax"])
            P.op("dve", lambda e: e.tensor_reduce(out=stb[:, 1:2], in_=sc[:, 0:N], axis=AX.X, op=ALU.min), reads=[ksc], writes=["rmin"])
            yield
        P.op("dve", lambda e: e.tensor_tensor(out=sc[:, N - 128:N], in0=sc[:, N - 128:N], in1=causal, op=ALU.add),
             reads=[ksc, "causal"], writes=[ksc])
        if i >= 2:
            P.op("dve", lambda e: e.tensor_tensor(out=stb[:, 2:3], in0=stb[:, 0:1], in1=stb[:, 1:2], op=ALU.subtract), reads=["rmax", "rmin"], writes=["R"])
            P.op("dve", lambda e: e.tensor_scalar(out=wk, in0=pow2, scalar1=stb[:, 2:3], scalar2=None, op0=ALU.mult), reads=["R", "pow2"], writes=["wk"])
            P.op("dve", lambda e: e.tensor_tensor(out=stb[:, 3:4], in0=wk[:, 1:2], in1=stb[:, 1:2], op=ALU.add), reads=["wk", "rmin"], writes=["mid"])
            for k in range(1, NBIS + 1):
                P.op("dve", lambda e: e.tensor_scalar(out=mb[par][:, 0:N], in0=sc[:, 0:N], scalar1=stb[:, 3:4], scalar2=None, op0=ALU.is_ge, op1=ALU.add,
                                                       accum_out=stb[:, 4:5]),
                     reads=[ksc, "mid"], writes=["mb%d" % par, "cnt"])
                last = (k == NBIS)
                P.op("dve", lambda e, last=last: e.tensor_scalar(out=stb[:, 5:6], in0=stb[:, 4:5], scalar1=TOPK - 0.5, scalar2=(1.0 if last else 0.5),
                                                                  op0=ALU.is_ge, op1=ALU.subtract),
                     reads=["cnt"], writes=["tq"])
                dst = thr[par] if last else stb[:, 3:4]
                P.op("dve", lambda e, k=k, dst=dst: e.scalar_tensor_tensor(out=dst, in0=stb[:, 5:6], scalar=wk[:, k:k + 1], op0=ALU.mult, in1=stb[:, 3:4], op1=ALU.add),
                     reads=["tq", "wk", "mid"], writes=["thr%d" % par if last else "mid"])
                yield
        else:
            P.op("dve", lambda e: e.memset(thr[par], -1e29), writes=["thr%d" % par])
        P.op("dve", lambda e: e.tensor_scalar(out=mb[par][:, 0:N], in0=sc[:, 0:N], scalar1=thr[par], scalar2=NEG, op0=ALU.is_lt, op1=ALU.mult),
             reads=[ksc, "thr%d" % par], writes=["mb%d" % par])
        yield

    def stageB(i):
        par = i % 2
        q3 = i % 3
        mx = mixed[i % 2]
        kmx = "mixed%d" % (i % 2)
        OB = [(5 + h // 3, (h % 3) * 129) for h in range(8)]
        P.dma("sp", pAt, pA_d[:, i * 128:(i + 1) * 128, :].rearrange("c p f -> p c f"), writes=["pAt"])
        totf = tot.rearrange("p h d -> p (h d)")
        P.op("pool", lambda e: e.tensor_tensor(out=totf, in0=pAt[:, 0, :], in1=pAt[:, 1, :], op=ALU.add), reads=["pAt"], writes=["tot"])
        P.op("pool", lambda e: e.tensor_tensor(out=totf, in0=totf, in1=pAt[:, 2, :], op=ALU.add), reads=["pAt", "tot"], writes=["tot"])

        def emit_pv(j):
            def pv(e, j=j):
                ins = None
                for h in range(8):
                    bank, col = OB[h]
                    ins = e.matmul(psf(bank)[:, col:col + 129], PT[j % 2][h // 4][:, (h % 4) * 128:(h % 4 + 1) * 128], ckv[:, j, :],
                                   start=(j == 0 and h % 3 == 0), stop=(j == i), skip_group_check=True)
                return ins
            P.op("pe", pv, reads=["PT%d0" % (j % 2), "PT%d1" % (j % 2)], writes=["ps5", "ps6", "ps7"])
        for j in range(i + 1):
            for half in range(2):
                def qk(e, j=j, half=half):
                    e.matmul(psf(3 + half), ckvT[:, j * 128:(j + 1) * 128], qlT[q3][:, half * 4:(half + 1) * 4, :], start=True, stop=False)
                    e.matmul(psf(3 + half), mb[par][:, j * 128:(j + 1) * 128], identrep, start=False, stop=False)
                    return e.matmul(psf(3 + half), alL[:, i - j, :], alR[:, half * 512:(half + 1) * 512], start=False, stop=True)
                P.op("pe", qk, reads=["qlTa%d" % q3, "qlTb%d" % q3, "mb%d" % par, "identrep", "alL", "alR"], writes=["ps%d" % (3 + half)])
                P.op("act", lambda e, j=j, half=half: e.activation(out=PT[j % 2][half], in_=psf(3 + half), func=AF.Exp, scale=0.125),
                     reads=["ps%d" % (3 + half)], writes=["PT%d%d" % (j % 2, half)])
            if j >= 1:
                emit_pv(j - 1)
            if j == 1:
                P.op("dve", lambda e: e.reciprocal(out=recA, in_=tot[:, :, 64]), reads=["tot"], writes=["recA"])
                P.op("dve", lambda e: e.tensor_tensor(out=mx[:, 0:512].rearrange("p (h d) -> p h d", d=64), in0=tot[:, :, 0:64],
                                                      in1=recA.unsqueeze(2).broadcast_to([128, 8, 64]), op=ALU.mult),
                     reads=["tot", "recA"], writes=[kmx + "a"])
            yield
        if i == 0:
            P.op("dve", lambda e: e.reciprocal(out=recA, in_=tot[:, :, 64]), reads=["tot"], writes=["recA"])
            P.op("dve", lambda e: e.tensor_tensor(out=mx[:, 0:512].rearrange("p (h d) -> p h d", d=64), in0=tot[:, :, 0:64],
                                                  in1=recA.unsqueeze(2).broadcast_to([128, 8, 64]), op=ALU.mult),
                 reads=["tot", "recA"], writes=[kmx + "a"])
        emit_pv(i)
        yield
        for b in range(3):
            n = 3 if b < 2 else 2
            ov = psf(5 + b)[:, 0:n * 129].rearrange("p (h c) -> p h c", c=129)
            P.op("dve", lambda e, b=b, n=n, ov=ov: e.tensor_scalar(out=rec[:, 3 * b:3 * b + n], in0=ov[:, :, 128], scalar1=1e-30, scalar2=None, op0=ALU.max),
                 reads=["ps%d" % (5 + b)], writes=["rec%d" % b])
            P.op("dve", lambda e, b=b, n=n: e.reciprocal(out=rec[:, 3 * b:3 * b + n], in_=rec[:, 3 * b:3 * b + n]), reads=["rec%d" % b], writes=["rec%d" % b])
            P.op("dve", lambda e, b=b, n=n, ov=ov: e.tensor_tensor(out=olat[:, 3 * b:3 * b + n, :], in0=ov[:, :, 0:128],
                                                                    in1=rec[:, 3 * b:3 * b + n].unsqueeze(2).broadcast_to([128, n, 128]), op=ALU.mult),
                 reads=["ps%d" % (5 + b), "rec%d" % b], writes=["olat%d" % b])
        yield

        def tro(e):
            ins = None
            for h in range(8):
                ins = e.transpose(psh(3)[:, h * 128:(h + 1) * 128], olat[:, h, :], ident)
            return ins
        P.op("pe", tro, reads=["olat0", "olat1", "olat2"], writes=["ps3"])
        yield
        P.op("act", lambda e: e.copy(out=olatT, in_=psh(3).rearrange("p (c t) -> p c t", t=128)), reads=["ps3"], writes=["olatT"])
        yield

        def mob(e):
            ins = None
            for h in range(8):
                ins = e.matmul(psf(4)[:, h * 64:(h + 1) * 64], olatT[:, h, :], wuv[:, h, :], start=True, stop=True)
            return ins
        P.op("pe", mob, reads=["olatT", "wuv"], writes=["ps4"])
        yield
        P.op("act", lambda e: e.copy(out=mx[:, 512:1024], in_=psf(4)), reads=["ps4"], writes=[kmx + "b"])
        P.dma("pool", mix_v[i], mx, reads=[kmx + "a", kmx + "b"], writes=["mix_d"])
        yield

    precast = list(L["precast"])
    wstage = [ar.alloc([128, 8, 704], BF16) for _ in range(2)]
    npc = 0
    pending = None
    for s_ in range(NT + 2):
        if pending is not None:
            P.dma("sp", pending[0], pending[1], reads=[pending[2]], writes=[pending[3]])
            pending = None
        if precast:
            o_, i_, k_ = precast.pop(0)
            stg = wstage[npc % 2]
            if k_.startswith("wdns"):
                stg = stg.rearrange("p a b -> p (a b)")[:, 0:2 * D].rearrange("p (a b) -> p a b", b=D)
            P.dma("pool", stg, i_, writes=["wstage%d" % (npc % 2)])
            pending = (o_, stg, "wstage%d" % (npc % 2), k_)
            npc += 1
        g1 = stageA1(s_) if s_ < NT else None
        g2 = stageA2(s_ - 1) if 1 <= s_ <= NT else None
        g3 = stageB(s_ - 2) if 2 <= s_ <= NT + 1 else None
        n1 = 5 + ((s_ + 4) // 4) * 5
        n2 = NBIS + 3
        n3 = s_ + 5
        interleave(g1, n1, g2, n2, g3, n3)
    if pending is not None:
        P.dma("sp", pending[0], pending[1], reads=[pending[2]], writes=[pending[3]])


def build(debug=False, phases=(0, 1, 2, 3)):
    from contextlib import ExitStack
    nc = bass.Bass("TRN2", target_bir_lowering=False)
    es = ExitStack()

    def din(name, shape):
        return nc.dram_tensor(name, shape, F32, kind="ExternalInput").ap()

    x_d = din("x", [T, D])
    c_d = din("c", [128, 8])
    wada_d = din("w_ada", [D, 6 * D])
    bada_d = din("b_ada", [1, 6 * D])
    gattn_d = din("g_attn", [1, D])
    win_d = din("w_in", [D, 2760])
    kvg_d = din("kv_norm_g", [1, 128])
    wuk_d = din("w_uk", [8, 64, 128])
    wuv_d = din("w_uv", [8, 128, 64])
    wout_d = din("w_out", [D, D])
    gffn_d = din("g_ffn", [1, D])
    wgu_d = din("w_gu", [D, 2 * DFF])
    wdown_d = din("w_down", [DFF, D])
    gfin_d = din("g_final", [1, D])
    consts = make_consts()
    cst_d = {k: din("k_" + k, list(v.shape)) for k, v in consts.items()}
    out_d = nc.dram_tensor("out", [T, D], F32, kind="ExternalOutput").ap()
    V_d = nc.dram_tensor("V_scr", [T, 520], BF16, kind="Internal").ap()
    pA_d = nc.dram_tensor("pA_scr", [3, T, 520], F32, kind="Internal").ap()
    x1_d = nc.dram_tensor("x1_scr", [T, D], F32, kind="Internal").ap()
    mix_d = nc.dram_tensor("mix_scr", [T, D], BF16, kind="Internal").ap()
    wgu_s = nc.dram_tensor("wgu_scr", [128, 8, 2 * DFF], BF16, kind="Internal").ap()
    wdn_s = nc.dram_tensor("wdn_scr", [128, NJ, D], BF16, kind="Internal").ap()
    WP = 704
    wgu_v = wgu_d.rearrange("(c p) n -> p c n", p=128)
    wdn_v = wdown_d.rearrange("(c p) n -> p c n", p=128)
    precast = []
    for pc in range(DFF // WP):
        for gu in range(2):
            c0 = gu * DFF + pc * WP
            precast.append((wgu_s[:, :, c0:c0 + WP], wgu_v[:, :, c0:c0 + WP], "wgus%d_%d" % (gu, pc)))
    for jp in range(0, NJ, 2):
        precast.append((wdn_s[:, jp:jp + 2, :], wdn_v[:, jp:jp + 2, :], "wdns%d" % (jp // 2)))
    dbg = {}
    if debug:
        dbg["pA"] = nc.dram_tensor("dbg_pA", [3, T, 520], F32, kind="ExternalOutput").ap()
        dbg["x1"] = nc.dram_tensor("dbg_x1", [T, D], F32, kind="ExternalOutput").ap()
        pA_d = dbg["pA"]
        x1_d = dbg["x1"]
        dbg["modB"] = nc.dram_tensor("dbg_modB", [128, 6 * D], F32, kind="ExternalOutput").ap()
        dbg["kaT"] = nc.dram_tensor("dbg_kaT", [128, 4 * T], BF16, kind="ExternalOutput").ap()
        dbg["ckv"] = nc.dram_tensor("dbg_ckv", [128, NT * 129], BF16, kind="ExternalOutput").ap()
        dbg["ckvT"] = nc.dram_tensor("dbg_ckvT", [128, T], BF16, kind="ExternalOutput").ap()

    ASZ = 207 * 1024
    arena_t = es.enter_context(nc.sbuf_tensor("arena", [128, ASZ], U8))
    ar = Arena(arena_t, ASZ)
    psb = [es.enter_context(nc.psum_tensor("ps%d" % i, [128, 512], F32)) for i in range(8)]
    P = Prog(nc, es)

    def psf(i):
        return psb[i][:, :]

    def psh(i):
        return psb[i][:, :].bitcast(BF16)

    modB2 = ar.alloc([128, 4 * D], F32)
    gfB = ar.alloc([128, D], F32)
    ident = ar.alloc([128, 128], BF16)
    p3_off = ar.off
    modB1 = ar.alloc([128, 2 * D], F32)
    SH1, GM1 = [modB1[:, i * D:(i + 1) * D] for i in range(2)]
    SH2, GM2, GA2, GA1 = [modB2[:, i * D:(i + 1) * D] for i in range(4)]
    persist_off = ar.off

    def modcol(j):
        if j < 4:
            return modB1[:, j * 512:(j + 1) * 512]
        if j < 6:
            return modB2[:, 3 * D + (j - 4) * 512:3 * D + (j - 3) * 512]
        return modB2[:, (j - 6) * 512:(j - 5) * 512]

    P.dma("pool", ident, cst_d["ident"], writes=["ident"])
    P.dma("sp", gfB, gfin_d.broadcast_to([128, D]), writes=["gfB"])
    W1C = 1728
    W1_OFF = ASZ - 28 * 1024
    w1, _ = ar.alloc_at(W1_OFF, [128, 8, W1C], BF16)
    if 1 in phases:
        win_v1 = win_d.rearrange("(c p) n -> p c n", p=128)
        for (d0, s0, n) in ((0, 0, 768), (768, 768, 768), (1536, 2048, 128), (1664, 2688, 64)):
            P.dma("pool", w1[:, :, d0:d0 + n], win_v1[:, :, s0:s0 + n], writes=["w1_%d" % d0])

    if 0 in phases:
        baB = ar.alloc([128, 6 * D], F32)
        g1B = ar.alloc([128, D], F32)
        g2B = ar.alloc([128, D], F32)
        cT = ar.alloc([128, 8], F32)
        ca = ar.alloc([128, 8], F32)
        CA = ar.alloc([128, 8, 128], F32)
        wa = [ar.alloc([128, 8, 512], F32) for _ in range(4)]
        P.dma("sp", cT, c_d, writes=["cT"])
        P.dma("sp", baB, bada_d.broadcast_to([128, 6 * D]), writes=["baB"])
        P.dma("sp", g1B, gattn_d.broadcast_to([128, D]), writes=["g1B"])
        P.dma("sp", g2B, gffn_d.broadcast_to([128, D]), writes=["g2B"])
        P.op("act", lambda e: e.activation(out=ca, in_=cT, func=AF.Silu), reads=["cT"], writes=["ca"])
        P.op("dve", lambda e: e.tensor_copy(out=CA, in_=ca.unsqueeze(2).broadcast_to([128, 8, 128])),
             reads=["ca"], writes=["CA"])
        wada_v = wada_d.rearrange("(c p) n -> p c n", p=128)
        for j in range(12):
            wt = wa[j % 4]
            P.dma("sp", wt, wada_v[:, :, j * 512:(j + 1) * 512], writes=["wa%d" % (j % 4)])
            pb = j % 2

            def mm(e, wt=wt, pb=pb):
                ins = None
                for kc in range(8):
                    ins = e.matmul(psf(pb), CA[:, kc, :], wt[:, kc, :], start=(kc == 0), stop=(kc == 7))
                return ins
            P.op("pe", mm, reads=["CA", "wa%d" % (j % 4)], writes=["ps%d" % pb])
            P.op("dve", lambda e, j=j, pb=pb: e.tensor_tensor(out=modcol(j), in0=psf(pb),
                                                               in1=baB[:, j * 512:(j + 1) * 512], op=ALU.add),
                 reads=["ps%d" % pb, "baB"], writes=["modB"])
        P.op("dve", lambda e: e.scalar_tensor_tensor(out=GM1, in0=GM1, scalar=1.0, op0=ALU.add, in1=g1B, op1=ALU.mult),
             reads=["modB", "g1B"], writes=["modB"])
        P.op("dve", lambda e: e.scalar_tensor_tensor(out=GM2, in0=GM2, scalar=1.0, op0=ALU.add, in1=g2B, op1=ALU.mult),
             reads=["modB", "g2B"], writes=["modB"])
        assert ar.off <= W1_OFF, ("phase-0 buffers overlap w1", ar.off, W1_OFF)
        if debug:
            P.dma("sp", dbg["modB"][:, 0:2 * D], modB1, reads=["modB"], writes=["dbg_modB"])
            P.dma("sp", dbg["modB"][:, 2 * D:3 * D], GA1, reads=["modB"], writes=["dbg_modB3"])
            P.dma("sp", dbg["modB"][:, 3 * D:6 * D], modB2[:, 0:3 * D], reads=["modB"], writes=["dbg_modB2"])
        P.barrier()
    ar.off = persist_off

    W1C = 1728
    kiT = ar.alloc([128, T], BF16)
    ckv = ar.alloc([128, NT, 129], BF16)
    ckvT = ar.alloc([128, T], BF16)
    p2b_off = ar.off
    qaT = ar.alloc([128, 4, T], BF16)
    kaT = ar.alloc([128, 4, T], BF16)
    p2a_off = ar.off
    if 1 in phases:
        kvgB = ar.alloc([128, 128], F32)
        RING = 3
        xt = [ar.alloc([128, D], F32) for _ in range(RING)]
        junk = ar.alloc([128, D], BF16)
        tmpf = [ar.alloc([128, D], F32) for _ in range(RING)]
        hb = [ar.alloc([128, D], BF16) for _ in range(RING)]
        hT = [ar.alloc([128, 8, 512], BF16) for _ in range(2)]
        vt = [ar.alloc([128, 8, 65], BF16) for _ in range(RING)]
        stR = [ar.alloc([128, 8], F32) for _ in range(RING)]
        nh1 = ar.alloc([128, 1], F32)
        assert ar.off <= W1_OFF, ("phase-1 buffers overlap w1", ar.off, W1_OFF)
        P.dma("sp", kvgB, kvg_d.broadcast_to([128, 128]), writes=["kvgB"])
        P.op("pool", lambda e: e.memset(nh1, -0.5), writes=["nhalf"])
        P.op("pool", lambda e: e.memset(ckv[:, :, 128:129], 1.0), writes=["ckv_ones"])
        P.op("pool", lambda e: e.memset(kiT[64:128, :], 0.0), writes=["kiT_z"])
        for i in range(RING):
            P.op("pool", lambda e, i=i: e.memset(vt[i][:, :, 64:65], 1.0), writes=["vt%d" % i])
        x_v = x_d.rearrange("(n p) d -> n p d", p=128)
        V_v = V_d.rearrange("(n p) c -> n p c", p=128)

        def tile1(t, part):
            g, tl = t // 4, t % 4
            hTg = hT[g % 2]
            kh = "hT%d" % (g % 2)
            s = t % RING
            st = stR[s]
            ks = "st%d_" % s
            pt_ = 0 if t % 2 == 0 else 7
            if part == 2:
                yield from tile1y(t, g, tl, hTg, kh, s, st, ks)
                return
            if part == 1:
                yield from tile1x2(t, tl, hTg, kh, s, pt_)
                return
            P.dma("sp", xt[s], x_v[t], writes=["xt%d" % s])
            P.op("act", lambda e: e.activation(out=junk, in_=xt[s], func=AF.Square, accum_out=st[:, 0:1]),
                 reads=["xt%d" % s], writes=["junk", ks + "0"])
            yield
            rstd_ops(P, st[:, 0:1], st[:, 1:2], st[:, 2:3], nh1, 1.0 / D, (ks + "0", ks + "1", ks + "2"))
            yield
            P.op("dve", lambda e: e.scalar_tensor_tensor(out=tmpf[s], in0=xt[s], scalar=st[:, 2:3], op0=ALU.mult, in1=GM1, op1=ALU.mult),
                 reads=["xt%d" % s, ks + "2", "modB"], writes=["tmpf%d" % s])
            yield

        def tile1x2(t, tl, hTg, kh, s, pt_):
            P.op("pool", lambda e: e.tensor_tensor(out=hb[s], in0=tmpf[s], in1=SH1, op=ALU.add),
                 reads=["tmpf%d" % s, "modB"], writes=["hb%d" % s])
            yield

            def tr(e):
                ins = None
                for c in range(8):
                    ins = e.transpose(psh(pt_)[:, c * 128:(c + 1) * 128], hb[s][:, c * 128:(c + 1) * 128], ident)
                return ins
            P.op("pe", tr, reads=["hb%d" % s, "ident"], writes=["ps%d" % pt_])
            yield
            P.op("act", lambda e: e.copy(out=hTg[:, :, tl * 128:(tl + 1) * 128], in_=psh(pt_).rearrange("p (c t) -> p c t", t=128)),
                 reads=["ps%d" % pt_], writes=[kh])
            yield

        def tile1y(t, g, tl, hTg, kh, s, st, ks):
            def mmv(e):
                ins = None
                for kc in range(8):
                    ins = e.matmul(psf(1), hTg[:, kc, tl * 128:(tl + 1) * 128], w1[:, kc, 1024:1536], start=(kc == 0), stop=(kc == 7))
                return ins
            P.op("pe", mmv, reads=[kh, "w1"], writes=["ps1"])

            def mmc(e):
                ins = None
                for kc in range(8):
                    ins = e.matmul(psf(2)[:, 0:128], hTg[:, kc, tl * 128:(tl + 1) * 128], w1[:, kc, 1536:1664], start=(kc == 0), stop=(kc == 7))
                return ins
            P.op("pe", mmc, reads=[kh, "w1"], writes=["ps2"])
            yield
            P.op("dve", lambda e: e.tensor_copy(out=vt[s][:, :, 0:64], in_=psf(1).rearrange("p (h d) -> p h d", d=64)),
                 reads=["ps1"], writes=["vt%d" % s])
            P.dma("sp", V_v[t], vt[s].rearrange("p h d -> p (h d)"), reads=["vt%d" % s], writes=["V_d"])
            P.op("act", lambda e: e.activation(out=junk[:, 0:128], in_=psf(2)[:, 0:128], func=AF.Square, accum_out=st[:, 3:4]),
                 reads=["ps2"], writes=["junk", ks + "3"])
            yield
            rstd_ops(P, st[:, 3:4], st[:, 4:5], st[:, 5:6], nh1, 1.0 / 128, (ks + "3", ks + "4", ks + "5"))
            yield
            P.op("dve", lambda e: e.scalar_tensor_tensor(out=ckv[:, t, 0:128], in0=psf(2)[:, 0:128], scalar=st[:, 5:6],
                                                         op0=ALU.mult, in1=kvgB, op1=ALU.mult),
                 reads=["ps2", ks + "5", "kvgB"], writes=["ckv%d" % t])
            yield
            P.op("pe", lambda e: e.transpose(psh(3)[:, 0:128], ckv[:, t, 0:128], ident), reads=["ckv%d" % t, "ident"], writes=["ps3"])
            yield
            P.op("act", lambda e: e.copy(out=ckvT[:, t * 128:(t + 1) * 128], in_=psh(3)[:, 0:128]), reads=["ps3"], writes=["ckvT%d" % t])
            yield
            if tl == 3:
                for ci in range(9):
                    pb = 4 + ci % 3
                    if ci < 8:
                        c0, M = ci * 128, 128
                    else:
                        c0, M = 1664, 64

                    def mmf(e, c0=c0, M=M, pb=pb):
                        ins = None
                        for kc in range(8):
                            ins = e.matmul(psf(pb)[0:M, :], w1[:, kc, c0:c0 + M], hTg[:, kc, :], start=(kc == 0), stop=(kc == 7))
                        return ins
                    P.op("pe", mmf, reads=[kh, "w1"], writes=["ps%d" % pb])
                    if ci < 4:
                        dst, key = qaT[:, ci, g * 512:(g + 1) * 512], "qaT"
                    elif ci < 8:
                        dst, key = kaT[:, ci - 4, g * 512:(g + 1) * 512], "kaT"
                    else:
                        dst, key = kiT[0:64, g * 512:(g + 1) * 512], "kiT"
                    if ci % 2 == 0:
                        P.op("act", lambda e, dst=dst, pb=pb, M=M: e.copy(out=dst, in_=psf(pb)[0:M, :]), reads=["ps%d" % pb], writes=[key + str(g)])
                    else:
                        P.op("dve", lambda e, dst=dst, pb=pb, M=M: e.tensor_copy(out=dst, in_=psf(pb)[0:M, :]), reads=["ps%d" % pb], writes=[key + str(g)])
                    yield

        for t in range(NT + 2):
            gx = tile1(t, 0) if t < NT else None
            gx2 = tile1(t - 1, 1) if 1 <= t <= NT else None
            gy = tile1(t - 2, 2) if t >= 2 else None
            interleave(gx, 3, gx2, 3, gy, 15 if (t - 2) % 4 == 3 else 6)
        if debug:
            P.barrier()
            P.dma("sp", dbg["kaT"], kaT.rearrange("p c t -> p (c t)"), writes=["dbg1"])
            P.dma("sp", dbg["ckv"], ckv.rearrange("p c t -> p (c t)"), writes=["dbg2"])
            P.dma("sp", dbg["ckvT"], ckvT, writes=["dbg3"])
        P.barrier()

    if 2 in phases:
        ar.off = p2a_off
        phase2a(P, ar, psf, psh, qaT, kaT, V_d, pA_d, cst_d)
        P.barrier()
        ar.off = p2b_off
        phase2b(P, ar, psf, psh, locals())
        P.barrier()
    else:
        xb = ar.alloc([128, D], F32)
        x_v = x_d.rearrange("(n p) d -> n p d", p=128)
        x1_v = x1_d.rearrange("(n p) d -> n p d", p=128)
        for t in range(NT):
            P.dma("sp", xb, x_v[t], writes=["xb"])
            P.dma("sp", x1_v[t], xb, reads=["xb"], writes=["x1_d"])
        P.barrier()

    ar.off = p3_off
    if 3 in phases:
        GT = 256
        NTG = GT // 128
        wgu = ar.alloc([128, 8, 2 * DFF], BF16)
        wdn = ar.alloc([128, NJ, D], BF16)
        p3w_off = ar.off
        woutc = ar.alloc([128, 8, D], BF16)
        mixt = [ar.alloc([128, D], BF16) for _ in range(3)]
        mixT = [ar.alloc([128, 8, 128], BF16) for _ in range(2)]
        xin = [ar.alloc([128, D], F32) for _ in range(3)]
        tmpc = ar.alloc([128, D], F32)
        x1o = [ar.alloc([128, D], F32) for _ in range(2)]
        ar.off = p3w_off
        x1t = [ar.alloc([128, D], F32) for _ in range(4)]
        h2 = ar.alloc([128, D], BF16)
        tmpA = ar.alloc([128, D], F32)
        h2T = [ar.alloc([128, 8, GT], BF16) for _ in range(2)]
        actT = ar.alloc([128, NJ, GT], BF16)
        sg = [ar.alloc([128, GT], F32) for _ in range(2)]
        x2 = ar.alloc([128, D], F32)
        outt = ar.alloc([128, D], F32)
        junk3 = ar.alloc([128, D], BF16)
        st3 = ar.alloc([128, 8], F32)
        nh3 = ar.alloc([128, 1], F32)
        P.dma("pool", woutc, wout_d.rearrange("(c p) n -> p c n", p=128), writes=["woutc"])
        wloads = []
        for pc in range(DFF // WP):
            for gu in range(2):
                c0 = gu * DFF + pc * WP
                wloads.append((wgu[:, :, c0:c0 + WP], wgu_s[:, :, c0:c0 + WP], "wgu%d_%d" % (gu, pc)))
        for jp in range(0, NJ, 2):
            wloads.append((wdn[:, jp:jp + 2, :], wdn_s[:, jp:jp + 2, :], "wdn%d" % (jp // 2)))
        assert 2 in phases
        x1_v = x1_d.rearrange("(n p) d -> n p d", p=128)
        out_v = out_d.rearrange("(n p) d -> n p d", p=128)
        if 2 in phases:
            x_v3 = x_d.rearrange("(n p) d -> n p d", p=128)
            mix_v3 = mix_d.rearrange("(n p) d -> n p d", p=128)
            def c2l(t):
                r3 = t % 3
                if wloads:
                    o_, i_, k_ = wloads.pop(0)
                    P.dma("sp", o_, i_, writes=[k_])
                P.dma("sp", mixt[r3], mix_v3[t], writes=["mixt%d" % r3])
                P.dma("sp", xin[r3], x_v3[t], writes=["xin%d" % r3])
                yield

            def c2x(t):
                s = t % 2
                r3 = t % 3

                def trm(e):
                    ins = None
                    for c in range(8):
                        ins = e.transpose(psh(t % 2)[:, c * 128:(c + 1) * 128], mixt[r3][:, c * 128:(c + 1) * 128], ident)
                    return ins
                P.op("pe", trm, reads=["mixt%d" % r3], writes=["ps%d" % (t % 2)])
                yield
                P.op("act", lambda e: e.copy(out=mixT[s], in_=psh(t % 2).rearrange("p (c t) -> p c t", t=128)), reads=["ps%d" % (t % 2)], writes=["mixT%d" % s])
                yield

            def c2y(t):
                s = t % 2
                r3 = t % 3
                for half in range(2):
                    def my(e, half=half):
                        ins = None
                        for c in range(8):
                            ins = e.matmul(psf(2 + half), mixT[s][:, c, :], woutc[:, c, half * 512:(half + 1) * 512], start=(c == 0), stop=(c == 7))
                        return ins
                    P.op("pe", my, reads=["mixT%d" % s, "woutc"], writes=["ps%d" % (2 + half)])
                    P.op("dve", lambda e, half=half: e.tensor_tensor(out=tmpc[:, half * 512:(half + 1) * 512], in0=psf(2 + half),
                                                                     in1=GA1[:, half * 512:(half + 1) * 512], op=ALU.mult),
                         reads=["ps%d" % (2 + half)], writes=["tmpc"])
                    yield
                P.op("pool", lambda e: e.tensor_tensor(out=x1o[s], in0=tmpc, in1=xin[r3], op=ALU.add), reads=["tmpc", "xin%d" % r3], writes=["x1o%d" % s])
                P.dma("pool", x1_v[t], x1o[s], reads=["x1o%d" % s], writes=["x1_d"])
                yield
            for t in range(NT + 2):
                interleave(c2l(t) if t < NT else None, 1, c2x(t - 1) if 1 <= t <= NT else None, 2, c2y(t - 2) if t >= 2 else None, 3)
            for o_, i_, k_ in wloads:
                P.dma("sp", o_, i_, writes=[k_])
            wloads = []
            P.barrier()
        for o_, i_, k_ in wloads:
            P.dma("sp", o_, i_, writes=[k_])
        P.op("pool", lambda e: e.memset(nh3, -0.5), writes=["nhalf"])

        def wkeys(gu, j):
            c0, c1 = j * 128, (j + 1) * 128 - 1
            return ["wgu%d_%d" % (gu, p) for p in range(c0 // WP, c1 // WP + 1)]

        def stageN(g):
            hTg = h2T[g % 2]
            kh = "h2T%d" % (g % 2)
            for tl in range(NTG):
                t = NTG * g + tl
                s = t % 4
                P.dma("sp", x1t[s], x1_v[t], reads=["x1_d"], writes=["x1t%d" % s])
                P.op("act", lambda e, s=s: e.activation(out=junk3, in_=x1t[s], func=AF.Square, accum_out=st3[:, 0:1]),
                     reads=["x1t%d" % s], writes=["junk3", "s30"])
                rstd_ops(P, st3[:, 0:1], st3[:, 1:2], st3[:, 2:3], nh3, 1.0 / D, ("s30", "s31", "s32"))
                yield
                P.op("dve", lambda e, s=s: e.scalar_tensor_tensor(out=tmpA, in0=x1t[s], scalar=st3[:, 2:3], op0=ALU.mult,
                                                                    in1=GM2, op1=ALU.mult),
                     reads=["x1t%d" % s, "s32", "modB"], writes=["tmpA"])
                yield
                P.op("pool", lambda e: e.tensor_tensor(out=h2, in0=tmpA, in1=SH2, op=ALU.add),
                     reads=["tmpA", "modB"], writes=["h2"])

                def tr(e):
                    ins = None
                    for c in range(8):
                        ins = e.transpose(psh(0)[:, c * 128:(c + 1) * 128], h2[:, c * 128:(c + 1) * 128], ident)
                    return ins
                P.op("pe", tr, reads=["h2", "ident"], writes=["ps0"])
                yield
                P.op("act", lambda e, tl=tl, hTg=hTg: e.copy(out=hTg[:, :, tl * 128:(tl + 1) * 128],
                                                              in_=psh(0).rearrange("p (c t) -> p c t", t=128)),
                     reads=["ps0"], writes=[kh])
                yield

        def stageM(g):
            hTg = h2T[g % 2]
            kh = "h2T%d" % (g % 2)
            for j in range(NJ):
                pg = 1 + (j % 2) * 2
                pu = pg + 1

                def mg(e, j=j, pg=pg, pu=pu, hTg=hTg):
                    ins = None
                    for kc in range(8):
                        ins = e.matmul(psf(pg)[:, 0:GT], wgu[:, kc, j * 128:(j + 1) * 128], hTg[:, kc, :],
                                       start=(kc == 0), stop=(kc == 7))
                    for kc in range(8):
                        ins = e.matmul(psf(pu)[:, 0:GT], wgu[:, kc, DFF + j * 128:DFF + (j + 1) * 128], hTg[:, kc, :],
                                       start=(kc == 0), stop=(kc == 7))
                    return ins
                P.op("pe", mg, reads=[kh] + wkeys(0, j) + wkeys(1, j), writes=["ps%d" % pg, "ps%d" % pu])
                sj = sg[j % 2]
                P.op("act", lambda e, pg=pg, sj=sj: e.activation(out=sj, in_=psf(pg)[:, 0:GT], func=AF.Silu),
                     reads=["ps%d" % pg], writes=["sg%d" % (j % 2)])
                P.op("dve", lambda e, j=j, pu=pu, sj=sj: e.tensor_tensor(out=actT[:, j, :], in0=psf(pu)[:, 0:GT], in1=sj, op=ALU.mult),
                     reads=["ps%d" % pu, "sg%d" % (j % 2)], writes=["actT"])
                yield
            for tl in range(NTG):
                t = NTG * g + tl
                s = t % 4
                for hf in range(2):
                    pb = 5 + hf

                    def md(e, tl=tl, hf=hf, pb=pb):
                        ins = None
                        for j in range(NJ):
                            ins = e.matmul(psf(pb), actT[:, j, tl * 128:(tl + 1) * 128], wdn[:, j, hf * 512:(hf + 1) * 512],
                                           start=(j == 0), stop=(j == NJ - 1))
                        return ins
                    P.op("pe", md, reads=["actT"] + ["wdn%d" % k for k in range(NJ // 2)], writes=["ps%d" % pb])
                    P.op("dve", lambda e, hf=hf, pb=pb: e.tensor_tensor(out=x2[:, hf * 512:(hf + 1) * 512], in0=psf(pb),
                                                                         in1=GA2[:, hf * 512:(hf + 1) * 512], op=ALU.mult),
                         reads=["ps%d" % pb, "modB"], writes=["x2"])
                    yield
                P.op("pool", lambda e, s=s: e.tensor_tensor(out=x2, in0=x2, in1=x1t[s], op=ALU.add),
                     reads=["x2", "x1t%d" % s], writes=["x2"])
                P.op("act", lambda e: e.activation(out=junk3, in_=x2, func=AF.Square, accum_out=st3[:, 3:4]),
                     reads=["x2"], writes=["junk3", "s33"])
                rstd_ops(P, st3[:, 3:4], st3[:, 4:5], st3[:, 5:6], nh3, 1.0 / D, ("s33", "s34", "s35"))
                yield
                P.op("dve", lambda e: e.scalar_tensor_tensor(out=outt, in0=x2, scalar=st3[:, 5:6], op0=ALU.mult, in1=gfB, op1=ALU.mult),
                     reads=["x2", "s35", "gfB"], writes=["outt"])
                P.dma("sp", out_v[t], outt, reads=["outt"], writes=["out_d"])
                yield

        NG = T // GT
        for g in range(NG + 1):
            ga = stageN(g) if g < NG else None
            gb = stageM(g - 1) if g >= 1 else None
            interleave(ga, 4 * NTG, gb, NJ + 4 * NTG)
    P.barrier()
    P.emit()
    es.close()
    return nc, consts, list(dbg.keys())


_CACHE = {}


def kernel(x, c, w_ada, b_ada, g_attn, w_in, kv_norm_g, w_uk, w_uv, w_out, g_ffn, w_gu, w_down, g_final, _debug=False, _phases=(0, 1, 2, 3)):
    key = (bool(_debug), tuple(_phases))
    if key not in _CACHE:
        _CACHE[key] = build(debug=_debug, phases=_phases)
    nc, consts, dbgk = _CACHE[key]
    f = lambda a: np.ascontiguousarray(np.asarray(a, dtype=np.float32))
    x = f(x); c = f(c)
    shared = {
        "w_ada": f(w_ada)[0], "b_ada": f(b_ada)[0][None, :], "g_attn": f(g_attn)[0][None, :], "w_in": f(w_in)[0],
        "kv_norm_g": f(kv_norm_g)[0][None, :], "w_uk": f(w_uk)[0], "w_uv": f(w_uv)[0], "w_out": f(w_out)[0],
        "g_ffn": f(g_ffn)[0][None, :], "w_gu": f(w_gu)[0], "w_down": f(w_down)[0], "g_final": f(g_final)[None, :],
    }
    for k, v in consts.items():
        shared["k_" + k] = np.ascontiguousarray(v)
    in_maps = []
    for b in range(8):
        m = dict(shared)
        m["x"] = x[b]
        m["c"] = np.ascontiguousarray(c[b].reshape(8, 128).T)
        in_maps.append(m)
    res = run_bass_kernel_spmd(nc, in_maps, core_ids=list(range(8)))
    out = np.stack([np.asarray(r["out"], dtype=np.float32) for r in res.results], axis=0)
    if _debug:
        return out, res.results
    return out
```

```python
import numpy as np
import concourse.bass as bass
import concourse.mybir as mybir
from concourse.bass_utils import run_bass_kernel_spmd

F32 = mybir.dt.float32
BF16 = mybir.dt.bfloat16
U8 = mybir.dt.uint8
AF = mybir.ActivationFunctionType
ALU = mybir.AluOpType
AX = mybir.AxisListType

D = 1024
T = 4096
NT = T // 128
DFF = 2816
NJ = DFF // 128
EPS = 1e-6
NEG = -30000.0
IDX_SCALE = 512.0 ** -0.5
TOPK = 256
NBIS = 13

COMPUTE = ("pe", "act", "dve", "pool")
CH = 16384
NDMA = 8


class Prog:
    def __init__(self, nc, es):
        self.nc = nc
        self.es = es
        self.engs = {"pe": nc.tensor, "act": nc.scalar, "dve": nc.vector, "pool": nc.gpsimd, "sp": nc.sync}
        self.ops = {e: [] for e in self.engs}
        self.cnt = {}
        self.sems = {}
        self.last_w = {}
        self.readers = {}
        self.seen = {e: {} for e in self.engs}
        self.dma_n = {e: 0 for e in self.engs}

    def _sem(self, name):
        if name not in self.sems:
            self.sems[name] = self.es.enter_context(self.nc.semaphore("s_" + name))
        return self.sems[name]

    def _tok_wait(self, tok):
        src, idx = tok
        if src[0] == "dma":
            return (self._sem("d_%s_%d" % (src[1], src[2])), 16 * (idx + 1))
        return (self._sem("%s_%d" % (src[0], idx // CH)), idx % CH + 1)

    def _need(self, eng, tok, waits):
        if tok is None:
            return
        src, idx = tok
        if self.seen[eng].get(src, -1) >= idx:
            return
        self.seen[eng][src] = idx
        waits.append(self._tok_wait(tok))

    def _deps(self, eng, reads, writes, is_dma=False):
        waits = []
        for k in reads:
            tok = self.last_w.get(k)
            if tok is not None and not (tok[0] == ("pe",) and eng == "pe"):
                self._need(eng, tok, waits)
        for k in writes:
            tok = self.last_w.get(k)
            if tok is not None and (is_dma or tok[0] != (eng,)):
                self._need(eng, tok, waits)
            for r in self.readers.get(k, ()):
                if is_dma or r[0] != (eng,):
                    self._need(eng, r, waits)
        return waits

    def op(self, eng, fn, reads=(), writes=()):
        waits = self._deps(eng, reads, writes)
        idx = self.cnt.get(eng, 0)
        self.cnt[eng] = idx + 1
        tok = ((eng,), idx)
        inc = (self._sem("%s_%d" % (eng, idx // CH)), 1)
        self.ops[eng].append((waits, fn, inc))
        for k in reads:
            self.readers.setdefault(k, []).append(tok)
        for k in writes:
            self.last_w[k] = tok
            self.readers[k] = []
        return tok

    def dma(self, q, out, in_, reads=(), writes=()):
        waits = self._deps(q, reads, writes, is_dma=True)
        n = self.dma_n[q]
        self.dma_n[q] = n + 1
        slot = n % NDMA
        idx = n // NDMA
        src = ("dma", q, slot)
        if idx > 0:
            self._need(q, (src, idx - 1), waits)
        tok = (src, idx)
        inc = (self._sem("d_%s_%d" % (q, slot)), 16)
        self.ops[q].append((waits, lambda e: e.dma_start(out=out, in_=in_), inc))
        for k in reads:
            self.readers.setdefault(k, []).append(tok)
        for k in writes:
            self.last_w[k] = tok
            self.readers[k] = []
        return tok

    def barrier(self):
        toks = []
        for e in COMPUTE:
            if self.cnt.get(e, 0) > 0:
                toks.append(((e,), self.cnt[e] - 1))
        for q in self.engs:
            n = self.dma_n[q]
            for slot in range(min(n, NDMA)):
                cntslot = (n - slot + NDMA - 1) // NDMA
                if cntslot > 0:
                    toks.append((("dma", q, slot), cntslot - 1))
        for e in self.engs:
            waits = []
            for tok in toks:
                self._need(e, tok, waits)
            if waits:
                self.ops[e].append((waits, None, None))
        self.last_w = {}
        self.readers = {}

    def emit(self):
        with self.nc.Block() as block:
            def mk(ename):
                def body(eng):
                    for waits, fn, inc in self.ops[ename]:
                        for s, v in waits:
                            eng.wait_ge(s, v)
                        if fn is not None:
                            ins = fn(eng)
                            ins.then_inc(inc[0], inc[1])
                return body
            block.tensor(mk("pe"))
            block.scalar(mk("act"))
            block.vector(mk("dve"))
            block.gpsimd(mk("pool"))
            block.sync(mk("sp"))


class Arena:
    def __init__(self, t, size):
        self.t = t
        self.size = size
        self.off = 0

    def alloc_at(self, off, shape, dtype):
        save = self.off
        self.off = off
        v = self.alloc(shape, dtype)
        end = self.off
        self.off = save
        return v, end

    def alloc(self, shape, dtype):
        esz = mybir.dt.size(dtype)
        n = 1
        for s in shape[1:]:
            n *= s
        nbytes = (n * esz + 31) // 32 * 32
        assert self.off + nbytes <= self.size, ("arena overflow", self.off, nbytes, self.size)
        v = self.t[0:128, self.off:self.off + nbytes]
        if dtype != U8:
            v = v.bitcast(dtype)
        v = v[:, 0:n]
        self.off += nbytes
        if len(shape) == 3:
            v = v.rearrange("p (a b) -> p a b", b=shape[2])
        elif len(shape) == 4:
            v = v.rearrange("p (a b c) -> p a b c", b=shape[2], c=shape[3])
        if shape[0] != 128:
            v = v[0:shape[0]]
        return v


def alibi_slopes():
    s = 2.0 ** (-8.0 * (np.arange(16, dtype=np.float32) + 1.0) / 16)
    return s[0::2].astype(np.float32), s[1::2].astype(np.float32)


def make_consts():
    sa, sb = alibi_slopes()
    c = {}
    c["ident"] = np.eye(128, dtype=np.float32)
    c["identrep"] = np.tile(np.eye(128, dtype=np.float32), (1, 4))
    ik = np.arange(128)[:, None]
    iq = np.arange(128)[None, :]
    bA = np.zeros((128, 3, 2, 8, 128), np.float32)
    for ci, dil in enumerate((1, 4, 16)):
        for h in range(8):
            dprev = iq - ik + 128
            bA[:, ci, 0, h, :] = np.where(dprev <= 128, -sa[h] * dil * dprev, NEG)
            dcur = iq - ik
            bA[:, ci, 1, h, :] = np.where(dcur >= 0, -sa[h] * dil * dcur, NEG)
    c["biasA"] = bA.reshape(128, -1)
    al = np.zeros((3, 32, 128), np.float32)
    for d in range(32):
        al[0, d, :] = d
        al[1, d, :] = 1.0
        al[2, d, :] = np.arange(128)
    c["alL"] = al.reshape(3, -1)
    ar = np.zeros((3, 8, 128), np.float32)
    for h in range(8):
        ar[0, h, :] = -1024.0 * sb[h]
        ar[1, h, :] = -8.0 * sb[h] * np.arange(128)
        ar[2, h, :] = 8.0 * sb[h]
    c["alR"] = ar.reshape(3, -1)
    cm = np.where(np.arange(128)[None, :] <= np.arange(128)[:, None], 0.0, -1e30).astype(np.float32)
    c["causal"] = cm
    c["pow2"] = np.tile((2.0 ** -np.arange(0, NBIS + 2, dtype=np.float32))[None, :], (128, 1)).astype(np.float32)
    return c


def pipeline(gens, depth):
    active = []
    nxt = 0
    while active or nxt < len(gens):
        while len(active) < depth and nxt < len(gens):
            active.append(gens[nxt])
            nxt += 1
        for g in list(active):
            try:
                next(g)
            except StopIteration:
                active.remove(g)


def interleave(*gw):
    gens = [[g, max(1, n), 0] for g, n in zip(gw[0::2], gw[1::2]) if g is not None]
    while gens:
        best = min(gens, key=lambda x: x[2] / x[1])
        try:
            next(best[0])
            best[2] += 1
        except StopIteration:
            gens.remove(best)


def phase2a(P, ar, psf, psh, qaT, kaT, V_d, pA_d, cst_d):
    biasA = ar.alloc([128, 6, 8, 128], F32)
    Ssb = [[[ar.alloc([128, 4, 128], F32) for _ in range(2)] for _ in range(2)] for _ in range(2)]
    PT = [[[ar.alloc([128, 512], BF16) for _ in range(2)] for _ in range(2)] for _ in range(2)]
    Vb = [ar.alloc([128, 8, 65], BF16) for _ in range(3)]
    ob = [ar.alloc([128, 520], F32) for _ in range(2)]
    P.dma("sp", biasA.rearrange("p a h q -> p (a h q)"), cst_d["biasA"], writes=["biasA"])
    blocks = []
    for ci, dil in enumerate((1, 4, 16)):
        nb = T // dil // 128
        for r in range(dil):
            for n in range(nb):
                blocks.append((ci, dil, r, n))
    nblk = len(blocks)

    def tsl(dil, r, n):
        base = r + dil * 128 * n
        return slice(base, base + dil * 127 + 1, dil)

    for idx in range(nblk + 1):
        if idx < nblk:
            ci, dil, r, n = blocks[idx]
            bp = idx % 2
            vs = idx % 3
            P.dma("sp", Vb[vs].rearrange("p h d -> p (h d)"), V_d[tsl(dil, r, n), :], writes=["Vb%d" % vs])
            whichs = (1,) if n == 0 else (0, 1)
            for which in whichs:
                kn = n - 1 if which == 0 else n
                for par in range(2):
                    bank = which * 2 + par

                    def qk(e, dil=dil, r=r, n=n, kn=kn, par=par, bank=bank):
                        ins = None
                        p0 = par * 64
                        for hh in range(4):
                            ins = e.matmul(psf(bank)[:, hh * 128:(hh + 1) * 128],
                                           kaT[p0:p0 + 64, hh, tsl(dil, r, kn)], qaT[p0:p0 + 64, hh, tsl(dil, r, n)],
                                           start=True, stop=True)
                        return ins
                    P.op("pe", qk, writes=["ps%d" % bank])
                    sb = Ssb[bp][which][par]
                    P.op("dve", lambda e, sb=sb, bank=bank, ci=ci, which=which, par=par: e.scalar_tensor_tensor(
                        out=sb, in0=psf(bank).rearrange("p (a b) -> p a b", b=128), scalar=0.125, op0=ALU.mult,
                        in1=biasA[:, ci * 2 + which, par::2, :], op1=ALU.add),
                        reads=["ps%d" % bank, "biasA"], writes=["Ssb%d%d%d" % (bp, which, par)])
                    pt = PT[bp][which][par]
                    P.op("act", lambda e, sb=sb, pt=pt: e.activation(out=pt, in_=sb.rearrange("p a b -> p (a b)"), func=AF.Exp),
                         reads=["Ssb%d%d%d" % (bp, which, par)], writes=["PT%d%d%d" % (bp, which, par)])
        if idx >= 1:
            j = idx - 1
            ci, dil, r, n = blocks[j]
            bp = j % 2
            whichs = (1,) if n == 0 else (0, 1)
            oa, obk = (4, 5) if bp == 0 else (6, 7)

            def pv(e, bp=bp, j=j, whichs=whichs, oa=oa, obk=obk):
                ins = None
                for h in range(8):
                    par, hh = h % 2, h // 2
                    bank = oa if h < 4 else obk
                    col = (h % 4) * 65
                    for wi_, which in enumerate(whichs):
                        vsl = (j - 1) % 3 if which == 0 else j % 3
                        ins = e.matmul(psf(bank)[:, col:col + 65], PT[bp][which][par][:, hh * 128:(hh + 1) * 128],
                                       Vb[vsl][:, h, :], start=(wi_ == 0), stop=(wi_ == len(whichs) - 1))
                return ins
            rd = ["PT%d%d%d" % (bp, w, p) for w in whichs for p in range(2)] + ["Vb%d" % (j % 3)]
            if n > 0:
                rd.append("Vb%d" % ((j - 1) % 3))
            P.op("pe", pv, reads=rd, writes=["ps%d" % oa, "ps%d" % obk])
            o = ob[bp]
            P.op("act", lambda e, o=o, oa=oa: e.copy(out=o[:, 0:260], in_=psf(oa)[:, 0:260]), reads=["ps%d" % oa], writes=["ob%da" % bp])
            P.op("dve", lambda e, o=o, obk=obk: e.tensor_copy(out=o[:, 260:520], in_=psf(obk)[:, 0:260]), reads=["ps%d" % obk], writes=["ob%db" % bp])
            P.dma("sp", pA_d[ci, tsl(dil, r, n), :], o, reads=["ob%da" % bp, "ob%db" % bp], writes=["pA_d"])


def rstd_ops(P, ss, ms, rstd, nhalf, scale, keys):
    k_ss, k_ms, k_r = keys
    P.op("dve", lambda e: e.tensor_scalar(out=ms, in0=ss, scalar1=scale, scalar2=EPS, op0=ALU.mult, op1=ALU.add), reads=[k_ss], writes=[k_ms])
    P.op("pool", lambda e: e.tensor_tensor(out=rstd, in0=ms, in1=nhalf, op=ALU.pow), reads=[k_ms, "nhalf"], writes=[k_r])


def phase2b(P, ar, psf, psh, L):
    x_d, mix_d, pA_d, cst_d = L["x_d"], L["mix_d"], L["pA_d"], L["cst_d"]
    win_d, wuk_d, wuv_d = L["win_d"], L["wuk_d"], L["wuv_d"]
    kiT, ckv, ckvT, ident = L["kiT"], L["ckv"], L["ckvT"], L["ident"]
    SH1, GM1 = L["SH1"], L["GM1"]
    w2 = ar.alloc([128, 8, 1032], BF16)
    wuk = ar.alloc([128, 4, 128], BF16)
    wuv = ar.alloc([128, 8, 64], BF16)
    identrep = ar.alloc([128, 512], BF16)
    alL = ar.alloc([128, 32, 128], BF16)
    alR = ar.alloc([128, 1024], BF16)
    causal = ar.alloc([128, 128], F32)
    pow2 = ar.alloc([128, NBIS + 2], F32)
    nhalf = ar.alloc([128, 1], F32)
    xq = ar.alloc([128, D], F32)
    junk = ar.alloc([128, D], BF16)
    hb = ar.alloc([128, D], BF16)
    tmpf = ar.alloc([128, D], F32)
    hTq = ar.alloc([128, 8, 128], BF16)
    qbT = ar.alloc([128, 4, 128], BF16)
    qis = ar.alloc([128, 8, 64], BF16)
    qiT = ar.alloc([128, 8, 128], BF16)
    wis = ar.alloc([128, 8], F32)
    cmax = [ar.alloc([128, 8], F32) for _ in range(2)]
    dg = ar.alloc([128, 8, 128], BF16)
    rl = [ar.alloc([128, 512], BF16) for _ in range(4)]
    st = ar.alloc([128, 8], F32)
    stb = ar.alloc([128, 8], F32)
    wk = ar.alloc([128, NBIS + 2], F32)
    score = [ar.alloc([128, T], F32) for _ in range(2)]
    mb = [ar.alloc([128, T], BF16) for _ in range(2)]
    qlT = [ar.alloc([128, 8, 128], BF16) for _ in range(3)]
    thr = [ar.alloc([128, 1], F32) for _ in range(2)]
    PT = [[ar.alloc([128, 512], BF16) for _ in range(2)] for _ in range(2)]
    rec = ar.alloc([128, 8], F32)
    recA = ar.alloc([128, 8], F32)
    olat = ar.alloc([128, 8, 128], BF16)
    olatT = ar.alloc([128, 8, 128], BF16)
    mixed = [ar.alloc([128, D], BF16) for _ in range(2)]
    pAt = ar.alloc([128, 3, 520], F32)
    tot = ar.alloc([128, 8, 65], F32)

    win_v = win_d.rearrange("(c p) n -> p c n", p=128)
    P.dma("pool", w2[:, :, 0:512], win_v[:, :, 1536:2048], writes=["w2"])
    P.dma("pool", w2[:, :, 512:1024], win_v[:, :, 2176:2688], writes=["w2"])
    P.dma("pool", w2[:, :, 1024:1032], win_v[:, :, 2752:2760], writes=["w2"])
    P.dma("pool", wuk, wuk_d.rearrange("(c two) d r -> (two d) c r", two=2), writes=["wuk"])
    P.dma("pool", wuv, wuv_d.rearrange("h r d -> r h d"), writes=["wuv"])
    P.dma("pool", identrep, cst_d["identrep"], writes=["identrep"])
    P.op("pool", lambda e: e.memset(alL, 0.0), writes=["alL"])
    P.op("pool", lambda e: e.memset(alR, 0.0), writes=["alR"])
    P.dma("pool", alL[0:3].rearrange("p a b -> p (a b)"), cst_d["alL"], writes=["alL"])
    P.dma("pool", alR[0:3], cst_d["alR"], writes=["alR"])
    P.dma("sp", causal, cst_d["causal"], writes=["causal"])
    P.dma("sp", pow2, cst_d["pow2"], writes=["pow2"])
    P.op("pool", lambda e: e.memset(nhalf, -0.5), writes=["nhalf"])
    P.op("pool", lambda e: e.memset(qiT[64:128], 0.0), writes=["qiT_z"])
    x_v = x_d.rearrange("(n p) d -> n p d", p=128)
    mix_v = mix_d.rearrange("(n p) d -> n p d", p=128)
    assert len(L["precast"]) <= NT + 2

    def stageA1(i):
        sp_ = i % 2
        q3 = i % 3
        sc = score[sp_]
        ksc = "score%d" % sp_
        N = 128 * (i + 1)
        P.dma("sp", xq, x_v[i], writes=["xq"])
        P.op("act", lambda e: e.activation(out=junk, in_=xq, func=AF.Square, accum_out=st[:, 0:1]), reads=["xq"], writes=["junk", "st0"])
        rstd_ops(P, st[:, 0:1], st[:, 1:2], st[:, 2:3], nhalf, 1.0 / D, ("st0", "st1", "st2"))
        P.op("dve", lambda e: e.scalar_tensor_tensor(out=tmpf, in0=xq, scalar=st[:, 2:3], op0=ALU.mult, in1=GM1, op1=ALU.mult),
             reads=["xq", "st2"], writes=["tmpf"])
        yield
        P.op("pool", lambda e: e.tensor_tensor(out=hb, in0=tmpf, in1=SH1, op=ALU.add), reads=["tmpf"], writes=["hb"])

        def tr(e):
            ins = None
            for c in range(8):
                ins = e.transpose(psh(0)[:, c * 128:(c + 1) * 128], hb[:, c * 128:(c + 1) * 128], ident)
            return ins
        P.op("pe", tr, reads=["hb"], writes=["ps0"])
        P.op("act", lambda e: e.copy(out=hTq, in_=psh(0).rearrange("p (c t) -> p c t", t=128)), reads=["ps0"], writes=["hTq"])
        yield

        def mqb(e):
            ins = None
            for cc in range(4):
                for kc in range(8):
                    ins = e.matmul(psf(1)[:, cc * 128:(cc + 1) * 128], w2[:, kc, cc * 128:(cc + 1) * 128], hTq[:, kc, :],
                                   start=(kc == 0), stop=(kc == 7))
            return ins
        P.op("pe", mqb, reads=["hTq", "w2"], writes=["ps1"])
        P.op("act", lambda e: e.copy(out=qbT, in_=psf(1).rearrange("p (c t) -> p c t", t=128)), reads=["ps1"], writes=["qbT"])

        def mqi(e):
            ins = None
            for kc in range(8):
                ins = e.matmul(psf(2), hTq[:, kc, :], w2[:, kc, 512:1024], start=(kc == 0), stop=(kc == 7))
            return ins
        P.op("pe", mqi, reads=["hTq", "w2"], writes=["ps2"])

        def mwi(e):
            ins = None
            for kc in range(8):
                ins = e.matmul(psf(0)[:, 0:8], hTq[:, kc, :], w2[:, kc, 1024:1032], start=(kc == 0), stop=(kc == 7))
            return ins
        P.op("pe", mwi, reads=["hTq", "w2"], writes=["ps0"])
        yield
        P.op("act", lambda e: e.activation(out=wis, in_=psf(0)[:, 0:8], func=AF.Copy, scale=IDX_SCALE), reads=["ps0"], writes=["wis"])
        P.op("dve", lambda e: e.tensor_tensor(out=dg, in0=ident.unsqueeze(1).broadcast_to([128, 8, 128]),
                                              in1=wis.unsqueeze(2).broadcast_to([128, 8, 128]), op=ALU.mult),
             reads=["wis"], writes=["dg"])
        P.op("act", lambda e: e.copy(out=qis, in_=psf(2).rearrange("p (h d) -> p h d", d=64)), reads=["ps2"], writes=["qis"])

        def tq(e):
            ins = None
            for s_ in range(8):
                ins = e.transpose(psh(2)[0:64, s_ * 128:(s_ + 1) * 128], qis[:, s_, :], ident)
            return ins
        P.op("pe", tq, reads=["qis"], writes=["ps2"])
        P.op("act", lambda e: e.copy(out=qiT[0:64], in_=psh(2)[0:64, :].rearrange("p (c t) -> p c t", t=128)), reads=["ps2"], writes=["qiT"])
        yield

        def mql(e):
            ins = None
            for h in range(8):
                p0 = (h % 2) * 64
                ins = e.matmul(psf(1 + h % 2)[:, (h // 2) * 128:(h // 2 + 1) * 128], wuk[p0:p0 + 64, h // 2, :], qbT[p0:p0 + 64, h // 2, :],
                               start=True, stop=True)
            return ins
        P.op("pe", mql, reads=["qbT", "wuk"], writes=["ps1", "ps2"])
        P.op("act", lambda e: e.copy(out=qlT[q3][:, 0::2, :], in_=psf(1).rearrange("p (c t) -> p c t", t=128)), reads=["ps1"], writes=["qlTa%d" % q3])
        P.op("dve", lambda e: e.tensor_copy(out=qlT[q3][:, 1::2, :], in_=psf(2).rearrange("p (c t) -> p c t", t=128)), reads=["ps2"], writes=["qlTb%d" % q3])
        yield
        nch = (N + 511) // 512
        for c in range(nch):
            W = min(512, N - 512 * c)
            ksl = slice(512 * c, 512 * c + W)

            def emit_L(h, ksl=ksl, W=W):
                b = 1 + h % 2
                P.op("pe", lambda e, h=h, b=b: e.matmul(psf(b)[:, 0:W], qiT[:, h, :], kiT[:, ksl], start=True, stop=True),
                     reads=["qiT"], writes=["ps%d" % b])
                rs = rl[h % 4]
                P.op("act", lambda e, b=b, rs=rs: e.activation(out=rs[:, 0:W], in_=psf(b)[:, 0:W], func=AF.Relu),
                     reads=["ps%d" % b], writes=["rl%d" % (h % 4)])

            def emit_D(h, W=W):
                rs = rl[h % 4]
                P.op("pe", lambda e, h=h, rs=rs: e.matmul(psf(0)[:, 0:W], dg[:, h, :], rs[:, 0:W], start=(h == 0), stop=(h == 7)),
                     reads=["dg", "rl%d" % (h % 4)], writes=["ps0"])
            emit_L(0)
            emit_L(1)
            for h in range(8):
                if h + 2 < 8:
                    emit_L(h + 2)
                emit_D(h)
                if h % 2 == 1:
                    yield
            P.op("dve", lambda e, ksl=ksl, W=W, c=c: e.tensor_scalar(out=sc[:, ksl], in0=psf(0)[:, 0:W], scalar1=1.0, scalar2=None, op0=ALU.mult, op1=ALU.max,
                                                                       accum_out=cmax[sp_][:, c:c + 1]),
                 reads=["ps0"], writes=[ksc, "cmax%d" % sp_])
            yield

    def stageA2(i):
        sp_ = i % 2
        par = i % 2
        sc = score[sp_]
        ksc = "score%d" % sp_
        N = 128 * (i + 1)
        nch = (N + 511) // 512
        if i >= 2:
            P.op("dve", lambda e: e.tensor_reduce(out=stb[:, 0:1], in_=cmax[sp_][:, 0:nch], axis=AX.X, op=ALU.max), reads=["cmax%d" % sp_], writes=["rmax"])
            P.op("dve", lambda e: e.tensor_reduce(out=stb[:, 1:2], in_=sc[:, 0:N], axis=AX.X, op=ALU.min), reads=[ksc], writes=["rmin"])
            yield
        P.op("dve", lambda e: e.tensor_tensor(out=sc[:, N - 128:N], in0=sc[:, N - 128:N], in1=causal, op=ALU.add),
             reads=[ksc, "causal"], writes=[ksc])
        if i >= 2:
            P.op("dve", lambda e: e.tensor_tensor(out=stb[:, 2:3], in0=stb[:, 0:1], in1=stb[:, 1:2], op=ALU.subtract), reads=["rmax", "rmin"], writes=["R"])
            P.op("dve", lambda e: e.tensor_scalar(out=wk, in0=pow2, scalar1=stb[:, 2:3], scalar2=None, op0=ALU.mult), reads=["R", "pow2"], writes=["wk"])
            P.op("dve", lambda e: e.tensor_tensor(out=stb[:, 3:4], in0=wk[:, 1:2], in1=stb[:, 1:2], op=ALU.add), reads=["wk", "rmin"], writes=["mid"])
            for k in range(1, NBIS + 1):
                P.op("dve", lambda e: e.tensor_scalar(out=mb[par][:, 0:N], in0=sc[:, 0:N], scalar1=stb[:, 3:4], scalar2=None, op0=ALU.is_ge, op1=ALU.add,
                                                       accum_out=stb[:, 4:5]),
                     reads=[ksc, "mid"], writes=["mb%d" % par, "cnt"])
                last = (k == NBIS)
                P.op("dve", lambda e, last=last: e.tensor_scalar(out=stb[:, 5:6], in0=stb[:, 4:5], scalar1=TOPK - 0.5, scalar2=(1.0 if last else 0.5),
                                                                  op0=ALU.is_ge, op1=ALU.subtract),
                     reads=["cnt"], writes=["tq"])
                dst = thr[par] if last else stb[:, 3:4]
                P.op("dve", lambda e, k=k, dst=dst: e.scalar_tensor_tensor(out=dst, in0=stb[:, 5:6], scalar=wk[:, k:k + 1], op0=ALU.mult, in1=stb[:, 3:4], op1=ALU.add),
                     reads=["tq", "wk", "mid"], writes=["thr%d" % par if last else "mid"])
                yield
        else:
            P.op("dve", lambda e: e.memset(thr[par], -1e29), writes=["thr%d" % par])
        P.op("dve", lambda e: e.tensor_scalar(out=mb[par][:, 0:N], in0=sc[:, 0:N], scalar1=thr[par], scalar2=NEG, op0=ALU.is_lt, op1=ALU.mult),
             reads=[ksc, "thr%d" % par], writes=["mb%d" % par])
        yield

    def stageB(i):
        par = i % 2
        q3 = i % 3
        mx = mixed[i % 2]
        kmx = "mixed%d" % (i % 2)
        OB = [(5 + h // 3, (h % 3) * 129) for h in range(8)]
        P.dma("sp", pAt, pA_d[:, i * 128:(i + 1) * 128, :].rearrange("c p f -> p c f"), writes=["pAt"])
        totf = tot.rearrange("p h d -> p (h d)")
        P.op("pool", lambda e: e.tensor_tensor(out=totf, in0=pAt[:, 0, :], in1=pAt[:, 1, :], op=ALU.add), reads=["pAt"], writes=["tot"])
        P.op("pool", lambda e: e.tensor_tensor(out=totf, in0=totf, in1=pAt[:, 2, :], op=ALU.add), reads=["pAt", "tot"], writes=["tot"])

        def emit_pv(j):
            def pv(e, j=j):
                ins = None
                for h in range(8):
                    bank, col = OB[h]
                    ins = e.matmul(psf(bank)[:, col:col + 129], PT[j % 2][h // 4][:, (h % 4) * 128:(h % 4 + 1) * 128], ckv[:, j, :],
                                   start=(j == 0 and h % 3 == 0), stop=(j == i), skip_group_check=True)
                return ins
            P.op("pe", pv, reads=["PT%d0" % (j % 2), "PT%d1" % (j % 2)], writes=["ps5", "ps6", "ps7"])
        for j in range(i + 1):
            for half in range(2):
                def qk(e, j=j, half=half):
                    e.matmul(psf(3 + half), ckvT[:, j * 128:(j + 1) * 128], qlT[q3][:, half * 4:(half + 1) * 4, :], start=True, stop=False)
                    e.matmul(psf(3 + half), mb[par][:, j * 128:(j + 1) * 128], identrep, start=False, stop=False)
                    return e.matmul(psf(3 + half), alL[:, i - j, :], alR[:, half * 512:(half + 1) * 512], start=False, stop=True)
                P.op("pe", qk, reads=["qlTa%d" % q3, "qlTb%d" % q3, "mb%d" % par, "identrep", "alL", "alR"], writes=["ps%d" % (3 + half)])
                P.op("act", lambda e, j=j, half=half: e.activation(out=PT[j % 2][half], in_=psf(3 + half), func=AF.Exp, scale=0.125),
                     reads=["ps%d" % (3 + half)], writes=["PT%d%d" % (j % 2, half)])
            if j >= 1:
                emit_pv(j - 1)
            if j == 1:
                P.op("dve", lambda e: e.reciprocal(out=recA, in_=tot[:, :, 64]), reads=["tot"], writes=["recA"])
                P.op("dve", lambda e: e.tensor_tensor(out=mx[:, 0:512].rearrange("p (h d) -> p h d", d=64), in0=tot[:, :, 0:64],
                                                      in1=recA.unsqueeze(2).broadcast_to([128, 8, 64]), op=ALU.mult),
                     reads=["tot", "recA"], writes=[kmx + "a"])
            yield
        if i == 0:
            P.op("dve", lambda e: e.reciprocal(out=recA, in_=tot[:, :, 64]), reads=["tot"], writes=["recA"])
            P.op("dve", lambda e: e.tensor_tensor(out=mx[:, 0:512].rearrange("p (h d) -> p h d", d=64), in0=tot[:, :, 0:64],
                                                  in1=recA.unsqueeze(2).broadcast_to([128, 8, 64]), op=ALU.mult),
                 reads=["tot", "recA"], writes=[kmx + "a"])
        emit_pv(i)
        yield
        for b in range(3):
            n = 3 if b < 2 else 2
            ov = psf(5 + b)[:, 0:n * 129].rearrange("p (h c) -> p h c", c=129)
            P.op("dve", lambda e, b=b, n=n, ov=ov: e.tensor_scalar(out=rec[:, 3 * b:3 * b + n], in0=ov[:, :, 128], scalar1=1e-30, scalar2=None, op0=ALU.max),
                 reads=["ps%d" % (5 + b)], writes=["rec%d" % b])
            P.op("dve", lambda e, b=b, n=n: e.reciprocal(out=rec[:, 3 * b:3 * b + n], in_=rec[:, 3 * b:3 * b + n]), reads=["rec%d" % b], writes=["rec%d" % b])
            P.op("dve", lambda e, b=b, n=n, ov=ov: e.tensor_tensor(out=olat[:, 3 * b:3 * b + n, :], in0=ov[:, :, 0:128],
                                                                    in1=rec[:, 3 * b:3 * b + n].unsqueeze(2).broadcast_to([128, n, 128]), op=ALU.mult),
                 reads=["ps%d" % (5 + b), "rec%d" % b], writes=["olat%d" % b])
        yield

        def tro(e):
            ins = None
            for h in range(8):
                ins = e.transpose(psh(3)[:, h * 128:(h + 1) * 128], olat[:, h, :], ident)
            return ins
        P.op("pe", tro, reads=["olat0", "olat1", "olat2"], writes=["ps3"])
        yield
        P.op("act", lambda e: e.copy(out=olatT, in_=psh(3).rearrange("p (c t) -> p c t", t=128)), reads=["ps3"], writes=["olatT"])
        yield

        def mob(e):
            ins = None
            for h in range(8):
                ins = e.matmul(psf(4)[:, h * 64:(h + 1) * 64], olatT[:, h, :], wuv[:, h, :], start=True, stop=True)
            return ins
        P.op("pe", mob, reads=["olatT", "wuv"], writes=["ps4"])
        yield
        P.op("act", lambda e: e.copy(out=mx[:, 512:1024], in_=psf(4)), reads=["ps4"], writes=[kmx + "b"])
        P.dma("pool", mix_v[i], mx, reads=[kmx + "a", kmx + "b"], writes=["mix_d"])
        yield

    precast = list(L["precast"])
    wstage = [ar.alloc([128, 8, 704], BF16) for _ in range(2)]
    npc = 0
    pending = None
    for s_ in range(NT + 2):
        if pending is not None:
            P.dma("sp", pending[0], pending[1], reads=[pending[2]], writes=[pending[3]])
            pending = None
        if precast:
            o_, i_, k_ = precast.pop(0)
            stg = wstage[npc % 2]
            if k_.startswith("wdns"):
                stg = stg.rearrange("p a b -> p (a b)")[:, 0:2 * D].rearrange("p (a b) -> p a b", b=D)
            P.dma("pool", stg, i_, writes=["wstage%d" % (npc % 2)])
            pending = (o_, stg, "wstage%d" % (npc % 2), k_)
            npc += 1
        g1 = stageA1(s_) if s_ < NT else None
        g2 = stageA2(s_ - 1) if 1 <= s_ <= NT else None
        g3 = stageB(s_ - 2) if 2 <= s_ <= NT + 1 else None
        n1 = 5 + ((s_ + 4) // 4) * 5
        n2 = NBIS + 3
        n3 = s_ + 5
        interleave(g1, n1, g2, n2, g3, n3)
    if pending is not None:
        P.dma("sp", pending[0], pending[1], reads=[pending[2]], writes=[pending[3]])


def build(debug=False, phases=(0, 1, 2, 3)):
    from contextlib import ExitStack
    nc = bass.Bass("TRN2", target_bir_lowering=False)
    es = ExitStack()

    def din(name, shape):
        return nc.dram_tensor(name, shape, F32, kind="ExternalInput").ap()

    x_d = din("x", [T, D])
    c_d = din("c", [128, 8])
    wada_d = din("w_ada", [D, 6 * D])
    bada_d = din("b_ada", [1, 6 * D])
    gattn_d = din("g_attn", [1, D])
    win_d = din("w_in", [D, 2760])
    kvg_d = din("kv_norm_g", [1, 128])
    wuk_d = din("w_uk", [8, 64, 128])
    wuv_d = din("w_uv", [8, 128, 64])
    wout_d = din("w_out", [D, D])
    gffn_d = din("g_ffn", [1, D])
    wgu_d = din("w_gu", [D, 2 * DFF])
    wdown_d = din("w_down", [DFF, D])
    gfin_d = din("g_final", [1, D])
    consts = make_consts()
    cst_d = {k: din("k_" + k, list(v.shape)) for k, v in consts.items()}
    out_d = nc.dram_tensor("out", [T, D], F32, kind="ExternalOutput").ap()
    V_d = nc.dram_tensor("V_scr", [T, 520], BF16, kind="Internal").ap()
    pA_d = nc.dram_tensor("pA_scr", [3, T, 520], F32, kind="Internal").ap()
    x1_d = nc.dram_tensor("x1_scr", [T, D], F32, kind="Internal").ap()
    mix_d = nc.dram_tensor("mix_scr", [T, D], BF16, kind="Internal").ap()
    wgu_s = nc.dram_tensor("wgu_scr", [128, 8, 2 * DFF], BF16, kind="Internal").ap()
    wdn_s = nc.dram_tensor("wdn_scr", [128, NJ, D], BF16, kind="Internal").ap()
    WP = 704
    wgu_v = wgu_d.rearrange("(c p) n -> p c n", p=128)
    wdn_v = wdown_d.rearrange("(c p) n -> p c n", p=128)
    precast = []
    for pc in range(DFF // WP):
        for gu in range(2):
            c0 = gu * DFF + pc * WP
            precast.append((wgu_s[:, :, c0:c0 + WP], wgu_v[:, :, c0:c0 + WP], "wgus%d_%d" % (gu, pc)))
    for jp in range(0, NJ, 2):
        precast.append((wdn_s[:, jp:jp + 2, :], wdn_v[:, jp:jp + 2, :], "wdns%d" % (jp // 2)))
    dbg = {}
    if debug:
        dbg["pA"] = nc.dram_tensor("dbg_pA", [3, T, 520], F32, kind="ExternalOutput").ap()
        dbg["x1"] = nc.dram_tensor("dbg_x1", [T, D], F32, kind="ExternalOutput").ap()
        pA_d = dbg["pA"]
        x1_d = dbg["x1"]
        dbg["modB"] = nc.dram_tensor("dbg_modB", [128, 6 * D], F32, kind="ExternalOutput").ap()
        dbg["kaT"] = nc.dram_tensor("dbg_kaT", [128, 4 * T], BF16, kind="ExternalOutput").ap()
        dbg["ckv"] = nc.dram_tensor("dbg_ckv", [128, NT * 129], BF16, kind="ExternalOutput").ap()
        dbg["ckvT"] = nc.dram_tensor("dbg_ckvT", [128, T], BF16, kind="ExternalOutput").ap()

    ASZ = 207 * 1024
    arena_t = es.enter_context(nc.sbuf_tensor("arena", [128, ASZ], U8))
    ar = Arena(arena_t, ASZ)
    psb = [es.enter_context(nc.psum_tensor("ps%d" % i, [128, 512], F32)) for i in range(8)]
    P = Prog(nc, es)

    def psf(i):
        return psb[i][:, :]

    def psh(i):
        return psb[i][:, :].bitcast(BF16)

    modB2 = ar.alloc([128, 4 * D], F32)
    gfB = ar.alloc([128, D], F32)
    ident = ar.alloc([128, 128], BF16)
    p3_off = ar.off
    modB1 = ar.alloc([128, 2 * D], F32)
    SH1, GM1 = [modB1[:, i * D:(i + 1) * D] for i in range(2)]
    SH2, GM2, GA2, GA1 = [modB2[:, i * D:(i + 1) * D] for i in range(4)]
    persist_off = ar.off

    def modcol(j):
        if j < 4:
            return modB1[:, j * 512:(j + 1) * 512]
        if j < 6:
            return modB2[:, 3 * D + (j - 4) * 512:3 * D + (j - 3) * 512]
        return modB2[:, (j - 6) * 512:(j - 5) * 512]

    P.dma("pool", ident, cst_d["ident"], writes=["ident"])
    P.dma("sp", gfB, gfin_d.broadcast_to([128, D]), writes=["gfB"])
    W1C = 1728
    W1_OFF = ASZ - 28 * 1024
    w1, _ = ar.alloc_at(W1_OFF, [128, 8, W1C], BF16)
    if 1 in phases:
        win_v1 = win_d.rearrange("(c p) n -> p c n", p=128)
        for (d0, s0, n) in ((0, 0, 768), (768, 768, 768), (1536, 2048, 128), (1664, 2688, 64)):
            P.dma("pool", w1[:, :, d0:d0 + n], win_v1[:, :, s0:s0 + n], writes=["w1_%d" % d0])

    if 0 in phases:
        baB = ar.alloc([128, 6 * D], F32)
        g1B = ar.alloc([128, D], F32)
        g2B = ar.alloc([128, D], F32)
        cT = ar.alloc([128, 8], F32)
        ca = ar.alloc([128, 8], F32)
        CA = ar.alloc([128, 8, 128], F32)
        wa = [ar.alloc([128, 8, 512], F32) for _ in range(4)]
        P.dma("sp", cT, c_d, writes=["cT"])
        P.dma("sp", baB, bada_d.broadcast_to([128, 6 * D]), writes=["baB"])
        P.dma("sp", g1B, gattn_d.broadcast_to([128, D]), writes=["g1B"])
        P.dma("sp", g2B, gffn_d.broadcast_to([128, D]), writes=["g2B"])
        P.op("act", lambda e: e.activation(out=ca, in_=cT, func=AF.Silu), reads=["cT"], writes=["ca"])
        P.op("dve", lambda e: e.tensor_copy(out=CA, in_=ca.unsqueeze(2).broadcast_to([128, 8, 128])),
             reads=["ca"], writes=["CA"])
        wada_v = wada_d.rearrange("(c p) n -> p c n", p=128)
        for j in range(12):
            wt = wa[j % 4]
            P.dma("sp", wt, wada_v[:, :, j * 512:(j + 1) * 512], writes=["wa%d" % (j % 4)])
            pb = j % 2

            def mm(e, wt=wt, pb=pb):
                ins = None
                for kc in range(8):
                    ins = e.matmul(psf(pb), CA[:, kc, :], wt[:, kc, :], start=(kc == 0), stop=(kc == 7))
                return ins
            P.op("pe", mm, reads=["CA", "wa%d" % (j % 4)], writes=["ps%d" % pb])
            P.op("dve", lambda e, j=j, pb=pb: e.tensor_tensor(out=modcol(j), in0=psf(pb),
                                                               in1=baB[:, j * 512:(j + 1) * 512], op=ALU.add),
                 reads=["ps%d" % pb, "baB"], writes=["modB"])
        P.op("dve", lambda e: e.scalar_tensor_tensor(out=GM1, in0=GM1, scalar=1.0, op0=ALU.add, in1=g1B, op1=ALU.mult),
             reads=["modB", "g1B"], writes=["modB"])
        P.op("dve", lambda e: e.scalar_tensor_tensor(out=GM2, in0=GM2, scalar=1.0, op0=ALU.add, in1=g2B, op1=ALU.mult),
             reads=["modB", "g2B"], writes=["modB"])
        assert ar.off <= W1_OFF, ("phase-0 buffers overlap w1", ar.off, W1_OFF)
        if debug:
            P.dma("sp", dbg["modB"][:, 0:2 * D], modB1, reads=["modB"], writes=["dbg_modB"])
            P.dma("sp", dbg["modB"][:, 2 * D:3 * D], GA1, reads=["modB"], writes=["dbg_modB3"])
            P.dma("sp", dbg["modB"][:, 3 * D:6 * D], modB2[:, 0:3 * D], reads=["modB"], writes=["dbg_modB2"])
        P.barrier()
    ar.off = persist_off

    W1C = 1728
    kiT = ar.alloc([128, T], BF16)
    ckv = ar.alloc([128, NT, 129], BF16)
    ckvT = ar.alloc([128, T], BF16)
    p2b_off = ar.off
    qaT = ar.alloc([128, 4, T], BF16)
    kaT = ar.alloc([128, 4, T], BF16)
    p2a_off = ar.off
    if 1 in phases:
        kvgB = ar.alloc([128, 128], F32)
        RING = 3
        xt = [ar.alloc([128, D], F32) for _ in range(4)]
        junk = ar.alloc([128, D], BF16)
        tmpf = [ar.alloc([128, D], F32) for _ in range(RING)]
        hb = [ar.alloc([128, D], BF16) for _ in range(RING)]
        hT = [ar.alloc([128, 8, 512], BF16) for _ in range(2)]
        vt = [ar.alloc([128, 8, 65], BF16) for _ in range(RING)]
        stR = [ar.alloc([128, 8], F32) for _ in range(RING)]
        nh1 = ar.alloc([128, 1], F32)
        assert ar.off <= W1_OFF, ("phase-1 buffers overlap w1", ar.off, W1_OFF)
        P.dma("sp", kvgB, kvg_d.broadcast_to([128, 128]), writes=["kvgB"])
        P.op("pool", lambda e: e.memset(nh1, -0.5), writes=["nhalf"])
        P.op("pool", lambda e: e.memset(ckv[:, :, 128:129], 1.0), writes=["ckv_ones"])
        P.op("pool", lambda e: e.memset(kiT[64:128, :], 0.0), writes=["kiT_z"])
        for i in range(RING):
            P.op("pool", lambda e, i=i: e.memset(vt[i][:, :, 64:65], 1.0), writes=["vt%d" % i])
        x_v = x_d.rearrange("(n p) d -> n p d", p=128)
        V_v = V_d.rearrange("(n p) c -> n p c", p=128)

        def tile1(t, part):
            g, tl = t // 4, t % 4
            hTg = hT[g % 2]
            kh = "hT%d" % (g % 2)
            s = t % RING
            st = stR[s]
            ks = "st%d_" % s
            pt_ = 0 if t % 2 == 0 else 7
            if part == 2:
                yield from tile1y(t, g, tl, hTg, kh, s, st, ks)
                return
            if part == 1:
                yield from tile1x2(t, tl, hTg, kh, s, pt_)
                return
            x4 = t % 4
            if part == 3:
                P.dma("sp", xt[x4], x_v[t], writes=["xt%d" % x4])
                yield
                return
            P.op("act", lambda e: e.activation(out=junk, in_=xt[x4], func=AF.Square, accum_out=st[:, 0:1]),
                 reads=["xt%d" % x4], writes=["junk", ks + "0"])
            yield
            rstd_ops(P, st[:, 0:1], st[:, 1:2], st[:, 2:3], nh1, 1.0 / D, (ks + "0", ks + "1", ks + "2"))
            yield
            P.op("dve", lambda e: e.scalar_tensor_tensor(out=tmpf[s], in0=xt[x4], scalar=st[:, 2:3], op0=ALU.mult, in1=GM1, op1=ALU.mult),
                 reads=["xt%d" % x4, ks + "2", "modB"], writes=["tmpf%d" % s])
            yield

        def tile1x2(t, tl, hTg, kh, s, pt_):
            P.op("pool", lambda e: e.tensor_tensor(out=hb[s], in0=tmpf[s], in1=SH1, op=ALU.add),
                 reads=["tmpf%d" % s, "modB"], writes=["hb%d" % s])
            yield

            def tr(e):
                ins = None
                for c in range(8):
                    ins = e.transpose(psh(pt_)[:, c * 128:(c + 1) * 128], hb[s][:, c * 128:(c + 1) * 128], ident)
                return ins
            P.op("pe", tr, reads=["hb%d" % s, "ident"], writes=["ps%d" % pt_])
            yield
            P.op("act", lambda e: e.copy(out=hTg[:, :, tl * 128:(tl + 1) * 128], in_=psh(pt_).rearrange("p (c t) -> p c t", t=128)),
                 reads=["ps%d" % pt_], writes=[kh])
            yield

        def tile1y(t, g, tl, hTg, kh, s, st, ks):
            def mmv(e):
                ins = None
                for kc in range(8):
                    ins = e.matmul(psf(1), hTg[:, kc, tl * 128:(tl + 1) * 128], w1[:, kc, 1024:1536], start=(kc == 0), stop=(kc == 7))
                return ins
            P.op("pe", mmv, reads=[kh, "w1"], writes=["ps1"])

            def mmc(e):
                ins = None
                for kc in range(8):
                    ins = e.matmul(psf(2)[:, 0:128], hTg[:, kc, tl * 128:(tl + 1) * 128], w1[:, kc, 1536:1664], start=(kc == 0), stop=(kc == 7))
                return ins
            P.op("pe", mmc, reads=[kh, "w1"], writes=["ps2"])
            yield
            P.op("dve", lambda e: e.tensor_copy(out=vt[s][:, :, 0:64], in_=psf(1).rearrange("p (h d) -> p h d", d=64)),
                 reads=["ps1"], writes=["vt%d" % s])
            P.dma("sp", V_v[t], vt[s].rearrange("p h d -> p (h d)"), reads=["vt%d" % s], writes=["V_d"])
            P.op("act", lambda e: e.activation(out=junk[:, 0:128], in_=psf(2)[:, 0:128], func=AF.Square, accum_out=st[:, 3:4]),
                 reads=["ps2"], writes=["junk", ks + "3"])
            yield
            rstd_ops(P, st[:, 3:4], st[:, 4:5], st[:, 5:6], nh1, 1.0 / 128, (ks + "3", ks + "4", ks + "5"))
            yield
            P.op("dve", lambda e: e.scalar_tensor_tensor(out=ckv[:, t, 0:128], in0=psf(2)[:, 0:128], scalar=st[:, 5:6],
                                                         op0=ALU.mult, in1=kvgB, op1=ALU.mult),
                 reads=["ps2", ks + "5", "kvgB"], writes=["ckv%d" % t])
            yield
            P.op("pe", lambda e: e.transpose(psh(3)[:, 0:128], ckv[:, t, 0:128], ident), reads=["ckv%d" % t, "ident"], writes=["ps3"])
            yield
            P.op("act", lambda e: e.copy(out=ckvT[:, t * 128:(t + 1) * 128], in_=psh(3)[:, 0:128]), reads=["ps3"], writes=["ckvT%d" % t])
            yield
            if tl == 3:
                for ci in range(9):
                    pb = 4 + ci % 3
                    if ci < 8:
                        c0, M = ci * 128, 128
                    else:
                        c0, M = 1664, 64

                    def mmf(e, c0=c0, M=M, pb=pb):
                        ins = None
                        for kc in range(8):
                            ins = e.matmul(psf(pb)[0:M, :], w1[:, kc, c0:c0 + M], hTg[:, kc, :], start=(kc == 0), stop=(kc == 7))
                        return ins
                    P.op("pe", mmf, reads=[kh, "w1"], writes=["ps%d" % pb])
                    if ci < 4:
                        dst, key = qaT[:, ci, g * 512:(g + 1) * 512], "qaT"
                    elif ci < 8:
                        dst, key = kaT[:, ci - 4, g * 512:(g + 1) * 512], "kaT"
                    else:
                        dst, key = kiT[0:64, g * 512:(g + 1) * 512], "kiT"
                    if ci % 2 == 0:
                        P.op("act", lambda e, dst=dst, pb=pb, M=M: e.copy(out=dst, in_=psf(pb)[0:M, :]), reads=["ps%d" % pb], writes=[key + str(g)])
                    else:
                        P.op("dve", lambda e, dst=dst, pb=pb, M=M: e.tensor_copy(out=dst, in_=psf(pb)[0:M, :]), reads=["ps%d" % pb], writes=[key + str(g)])
                    yield

        for t in range(NT + 3):
            gl = tile1(t, 3) if t < NT else None
            gx = tile1(t - 1, 0) if 1 <= t <= NT else None
            gx2 = tile1(t - 2, 1) if 2 <= t <= NT + 1 else None
            gy = tile1(t - 3, 2) if t >= 3 else None
            interleave(gl, 1, gx, 3, gx2, 3, gy, 15 if (t - 3) % 4 == 3 else 6)
        if debug:
            P.barrier()
            P.dma("sp", dbg["kaT"], kaT.rearrange("p c t -> p (c t)"), writes=["dbg1"])
            P.dma("sp", dbg["ckv"], ckv.rearrange("p c t -> p (c t)"), writes=["dbg2"])
            P.dma("sp", dbg["ckvT"], ckvT, writes=["dbg3"])
        P.barrier()

    if 2 in phases:
        ar.off = p2a_off
        phase2a(P, ar, psf, psh, qaT, kaT, V_d, pA_d, cst_d)
        P.barrier()
        ar.off = p2b_off
        phase2b(P, ar, psf, psh, locals())
        P.barrier()
    else:
        xb = ar.alloc([128, D], F32)
        x_v = x_d.rearrange("(n p) d -> n p d", p=128)
        x1_v = x1_d.rearrange("(n p) d -> n p d", p=128)
        for t in range(NT):
            P.dma("sp", xb, x_v[t], writes=["xb"])
            P.dma("sp", x1_v[t], xb, reads=["xb"], writes=["x1_d"])
        P.barrier()

    ar.off = p3_off
    if 3 in phases:
        GT = 256
        NTG = GT // 128
        wgu = ar.alloc([128, 8, 2 * DFF], BF16)
        wdn = ar.alloc([128, NJ, D], BF16)
        p3w_off = ar.off
        woutc = ar.alloc([128, 8, D], BF16)
        mixt = [ar.alloc([128, D], BF16) for _ in range(3)]
        mixT = [ar.alloc([128, 8, 128], BF16) for _ in range(2)]
        xin = [ar.alloc([128, D], F32) for _ in range(3)]
        tmpc = ar.alloc([128, D], F32)
        x1o = [ar.alloc([128, D], F32) for _ in range(2)]
        ar.off = p3w_off
        x1t = [ar.alloc([128, D], F32) for _ in range(4)]
        h2 = ar.alloc([128, D], BF16)
        tmpA = ar.alloc([128, D], F32)
        h2T = [ar.alloc([128, 8, GT], BF16) for _ in range(2)]
        actT = ar.alloc([128, NJ, GT], BF16)
        sg = [ar.alloc([128, GT], F32) for _ in range(2)]
        x2 = ar.alloc([128, D], F32)
        outt = ar.alloc([128, D], F32)
        junk3 = ar.alloc([128, D], BF16)
        st3 = ar.alloc([128, 8], F32)
        nh3 = ar.alloc([128, 1], F32)
        P.dma("pool", woutc, wout_d.rearrange("(c p) n -> p c n", p=128), writes=["woutc"])
        wloads = []
        for pc in range(DFF // WP):
            for gu in range(2):
                c0 = gu * DFF + pc * WP
                wloads.append((wgu[:, :, c0:c0 + WP], wgu_s[:, :, c0:c0 + WP], "wgu%d_%d" % (gu, pc)))
        for jp in range(0, NJ, 2):
            wloads.append((wdn[:, jp:jp + 2, :], wdn_s[:, jp:jp + 2, :], "wdn%d" % (jp // 2)))
        assert 2 in phases
        x1_v = x1_d.rearrange("(n p) d -> n p d", p=128)
        out_v = out_d.rearrange("(n p) d -> n p d", p=128)
        if 2 in phases:
            x_v3 = x_d.rearrange("(n p) d -> n p d", p=128)
            mix_v3 = mix_d.rearrange("(n p) d -> n p d", p=128)
            def c2l(t):
                r3 = t % 3
                if wloads:
                    o_, i_, k_ = wloads.pop(0)
                    P.dma("sp", o_, i_, writes=[k_])
                P.dma("sp", mixt[r3], mix_v3[t], writes=["mixt%d" % r3])
                P.dma("sp", xin[r3], x_v3[t], writes=["xin%d" % r3])
                yield

            def c2x(t):
                s = t % 2
                r3 = t % 3

                def trm(e):
                    ins = None
                    for c in range(8):
                        ins = e.transpose(psh(t % 2)[:, c * 128:(c + 1) * 128], mixt[r3][:, c * 128:(c + 1) * 128], ident)
                    return ins
                P.op("pe", trm, reads=["mixt%d" % r3], writes=["ps%d" % (t % 2)])
                yield
                P.op("act", lambda e: e.copy(out=mixT[s], in_=psh(t % 2).rearrange("p (c t) -> p c t", t=128)), reads=["ps%d" % (t % 2)], writes=["mixT%d" % s])
                yield

            def c2y(t):
                s = t % 2
                r3 = t % 3
                for half in range(2):
                    def my(e, half=half):
                        ins = None
                        for c in range(8):
                            ins = e.matmul(psf(2 + half), mixT[s][:, c, :], woutc[:, c, half * 512:(half + 1) * 512], start=(c == 0), stop=(c == 7))
                        return ins
                    P.op("pe", my, reads=["mixT%d" % s, "woutc"], writes=["ps%d" % (2 + half)])
                    P.op("dve", lambda e, half=half: e.tensor_tensor(out=tmpc[:, half * 512:(half + 1) * 512], in0=psf(2 + half),
                                                                     in1=GA1[:, half * 512:(half + 1) * 512], op=ALU.mult),
                         reads=["ps%d" % (2 + half)], writes=["tmpc"])
                    yield
                P.op("pool", lambda e: e.tensor_tensor(out=x1o[s], in0=tmpc, in1=xin[r3], op=ALU.add), reads=["tmpc", "xin%d" % r3], writes=["x1o%d" % s])
                P.dma("pool", x1_v[t], x1o[s], reads=["x1o%d" % s], writes=["x1_d"])
                yield
            for t in range(NT + 2):
                interleave(c2l(t) if t < NT else None, 1, c2x(t - 1) if 1 <= t <= NT else None, 2, c2y(t - 2) if t >= 2 else None, 3)
            for o_, i_, k_ in wloads:
                P.dma("sp", o_, i_, writes=[k_])
            wloads = []
            P.barrier()
        for o_, i_, k_ in wloads:
            P.dma("sp", o_, i_, writes=[k_])
        P.op("pool", lambda e: e.memset(nh3, -0.5), writes=["nhalf"])

        def wkeys(gu, j):
            c0, c1 = j * 128, (j + 1) * 128 - 1
            return ["wgu%d_%d" % (gu, p) for p in range(c0 // WP, c1 // WP + 1)]

        def stageN(g):
            hTg = h2T[g % 2]
            kh = "h2T%d" % (g % 2)
            for tl in range(NTG):
                t = NTG * g + tl
                s = t % 4
                P.dma("sp", x1t[s], x1_v[t], reads=["x1_d"], writes=["x1t%d" % s])
                P.op("act", lambda e, s=s: e.activation(out=junk3, in_=x1t[s], func=AF.Square, accum_out=st3[:, 0:1]),
                     reads=["x1t%d" % s], writes=["junk3", "s30"])
                rstd_ops(P, st3[:, 0:1], st3[:, 1:2], st3[:, 2:3], nh3, 1.0 / D, ("s30", "s31", "s32"))
                yield
                P.op("dve", lambda e, s=s: e.scalar_tensor_tensor(out=tmpA, in0=x1t[s], scalar=st3[:, 2:3], op0=ALU.mult,
                                                                    in1=GM2, op1=ALU.mult),
                     reads=["x1t%d" % s, "s32", "modB"], writes=["tmpA"])
                yield
                P.op("pool", lambda e: e.tensor_tensor(out=h2, in0=tmpA, in1=SH2, op=ALU.add),
                     reads=["tmpA", "modB"], writes=["h2"])

                def tr(e):
                    ins = None
                    for c in range(8):
                        ins = e.transpose(psh(0)[:, c * 128:(c + 1) * 128], h2[:, c * 128:(c + 1) * 128], ident)
                    return ins
                P.op("pe", tr, reads=["h2", "ident"], writes=["ps0"])
                yield
                P.op("act", lambda e, tl=tl, hTg=hTg: e.copy(out=hTg[:, :, tl * 128:(tl + 1) * 128],
                                                              in_=psh(0).rearrange("p (c t) -> p c t", t=128)),
                     reads=["ps0"], writes=[kh])
                yield

        def stageM(g):
            hTg = h2T[g % 2]
            kh = "h2T%d" % (g % 2)
            for j in range(NJ):
                pg = 1 + (j % 2) * 2
                pu = pg + 1

                def mg(e, j=j, pg=pg, pu=pu, hTg=hTg):
                    ins = None
                    for kc in range(8):
                        ins = e.matmul(psf(pg)[:, 0:GT], wgu[:, kc, j * 128:(j + 1) * 128], hTg[:, kc, :],
                                       start=(kc == 0), stop=(kc == 7))
                    for kc in range(8):
                        ins = e.matmul(psf(pu)[:, 0:GT], wgu[:, kc, DFF + j * 128:DFF + (j + 1) * 128], hTg[:, kc, :],
                                       start=(kc == 0), stop=(kc == 7))
                    return ins
                P.op("pe", mg, reads=[kh] + wkeys(0, j) + wkeys(1, j), writes=["ps%d" % pg, "ps%d" % pu])
                sj = sg[j % 2]
                P.op("act", lambda e, pg=pg, sj=sj: e.activation(out=sj, in_=psf(pg)[:, 0:GT], func=AF.Silu),
                     reads=["ps%d" % pg], writes=["sg%d" % (j % 2)])
                P.op("dve", lambda e, j=j, pu=pu, sj=sj: e.tensor_tensor(out=actT[:, j, :], in0=psf(pu)[:, 0:GT], in1=sj, op=ALU.mult),
                     reads=["ps%d" % pu, "sg%d" % (j % 2)], writes=["actT"])
                yield
            for tl in range(NTG):
                t = NTG * g + tl
                s = t % 4
                for hf in range(2):
                    pb = 5 + hf

                    def md(e, tl=tl, hf=hf, pb=pb):
                        ins = None
                        for j in range(NJ):
                            ins = e.matmul(psf(pb), actT[:, j, tl * 128:(tl + 1) * 128], wdn[:, j, hf * 512:(hf + 1) * 512],
                                           start=(j == 0), stop=(j == NJ - 1))
                        return ins
                    P.op("pe", md, reads=["actT"] + ["wdn%d" % k for k in range(NJ // 2)], writes=["ps%d" % pb])
                    P.op("dve", lambda e, hf=hf, pb=pb: e.tensor_tensor(out=x2[:, hf * 512:(hf + 1) * 512], in0=psf(pb),
                                                                         in1=GA2[:, hf * 512:(hf + 1) * 512], op=ALU.mult),
                         reads=["ps%d" % pb, "modB"], writes=["x2"])
                    yield
                P.op("pool", lambda e, s=s: e.tensor_tensor(out=x2, in0=x2, in1=x1t[s], op=ALU.add),
                     reads=["x2", "x1t%d" % s], writes=["x2"])
                P.op("act", lambda e: e.activation(out=junk3, in_=x2, func=AF.Square, accum_out=st3[:, 3:4]),
                     reads=["x2"], writes=["junk3", "s33"])
                rstd_ops(P, st3[:, 3:4], st3[:, 4:5], st3[:, 5:6], nh3, 1.0 / D, ("s33", "s34", "s35"))
                yield
                P.op("dve", lambda e: e.scalar_tensor_tensor(out=outt, in0=x2, scalar=st3[:, 5:6], op0=ALU.mult, in1=gfB, op1=ALU.mult),
                     reads=["x2", "s35", "gfB"], writes=["outt"])
                P.dma("sp", out_v[t], outt, reads=["outt"], writes=["out_d"])
                yield

        NG = T // GT
        for g in range(NG + 1):
            ga = stageN(g) if g < NG else None
            gb = stageM(g - 1) if g >= 1 else None
            interleave(ga, 4 * NTG, gb, NJ + 4 * NTG)
    P.barrier()
    P.emit()
    es.close()
    return nc, consts, list(dbg.keys())


_CACHE = {}


def kernel(x, c, w_ada, b_ada, g_attn, w_in, kv_norm_g, w_uk, w_uv, w_out, g_ffn, w_gu, w_down, g_final, _debug=False, _phases=(0, 1, 2, 3)):
    key = (bool(_debug), tuple(_phases))
    if key not in _CACHE:
        _CACHE[key] = build(debug=_debug, phases=_phases)
    nc, consts, dbgk = _CACHE[key]
    f = lambda a: np.ascontiguousarray(np.asarray(a, dtype=np.float32))
    x = f(x); c = f(c)
    shared = {
        "w_ada": f(w_ada)[0], "b_ada": f(b_ada)[0][None, :], "g_attn": f(g_attn)[0][None, :], "w_in": f(w_in)[0],
        "kv_norm_g": f(kv_norm_g)[0][None, :], "w_uk": f(w_uk)[0], "w_uv": f(w_uv)[0], "w_out": f(w_out)[0],
        "g_ffn": f(g_ffn)[0][None, :], "w_gu": f(w_gu)[0], "w_down": f(w_down)[0], "g_final": f(g_final)[None, :],
    }
    for k, v in consts.items():
        shared["k_" + k] = np.ascontiguousarray(v)
    in_maps = []
    for b in range(8):
        m = dict(shared)
        m["x"] = x[b]
        m["c"] = np.ascontiguousarray(c[b].reshape(8, 128).T)
        in_maps.append(m)
    res = run_bass_kernel_spmd(nc, in_maps, core_ids=list(range(8)))
    out = np.stack([np.asarray(r["out"], dtype=np.float32) for r in res.results], axis=0)
    if _debug:
        return out, res.results
    return out
```
